# Optimizing a Trainium2 kernel written in Bass

```python
import jax
import jax.numpy as jnp
from jax import lax
import numpy as np

D_MODEL = 1024
BATCH = 8
SEQ = 2048
DEPTH = 2

HEAD_DIM = 64
N_HEADS = 8
N_KV = 2
HPG = N_HEADS // N_KV
ATTN_W = N_HEADS * HEAD_DIM
KV_W = N_KV * HEAD_DIM
CONV_CH = D_MODEL - ATTN_W
CONV_W = 3
D_MIX = ATTN_W + CONV_CH
CMP_BLOCK = 32
CMP_STRIDE = 16
CMP_HIDDEN = 256
SEL_BLOCK = 64
SEL_TOPN = 16
WINDOW = 512
WIN_QBLOCK = 128
SEL_QCHUNK = 64
D_FF = 2816
N_EXPERTS = 8
TOP_K = 2
D_FF_EXPERT = 1408
N_DENSE = (DEPTH + 1) // 2
N_MOE = DEPTH // 2
ALPHA = (2 * DEPTH) ** 0.25
BETA = (8 * DEPTH) ** -0.25
LN_EPS = 1e-5
NEG = -1e30
FORCE = 1e9
SPLIT_WIDTHS = (ATTN_W, KV_W, KV_W, KV_W, KV_W, KV_W, KV_W, 3 * N_HEADS, CONV_CH, CONV_CH, CONV_CH)
D_IN = sum(SPLIT_WIDTHS)

kernel_name = 'hymba_nsa_shortconv_deepnorm_moe'


def layer_norm(x, g, b):
    xf = x.astype(jnp.float32)
    mu = jnp.mean(xf, -1, keepdims=True)
    var = jnp.mean(jnp.square(xf - mu), -1, keepdims=True)
    return ((xf - mu) * lax.rsqrt(var + LN_EPS) * g + b).astype(x.dtype)


def compress(k, pos, w1, b1, w2, b2):
    bsz, g, s, dh = k.shape
    ratio = CMP_BLOCK // CMP_STRIDE
    n_cmp = s // CMP_STRIDE - ratio + 1
    blocks = k.reshape(bsz, g, s // CMP_STRIDE, CMP_STRIDE, dh)
    win = jnp.concatenate([blocks[:, :, r:r + n_cmp] for r in range(ratio)], axis=3)
    win = (win + pos).reshape(bsz, g, n_cmp, CMP_BLOCK * dh)
    return jax.nn.gelu(win @ w1 + b1) @ w2 + b2


def cmp_attention(q, kc, vc):
    s_len = q.shape[3]
    n = kc.shape[2]
    t = jnp.arange(s_len)
    ends = jnp.arange(n) * CMP_STRIDE + CMP_BLOCK - 1
    mask = ends[None, :] <= t[:, None]
    sc = jnp.einsum('bghsd,bgnd->bghsn', q, kc).astype(jnp.float32) * HEAD_DIM ** -0.5
    p = jax.nn.softmax(jnp.where(mask, sc, NEG), axis=-1) * mask
    o = jnp.einsum('bghsn,bgnd->bghsd', p.astype(vc.dtype), vc)
    return o, p


def select_blocks(p_cmp, s_len):
    n = p_cmp.shape[-1]
    n_sel = s_len // SEL_BLOCK
    cs = jnp.arange(n) * CMP_STRIDE
    ss = jnp.arange(n_sel) * SEL_BLOCK
    overlap = ((cs[:, None] < ss[None, :] + SEL_BLOCK) & (cs[:, None] + CMP_BLOCK > ss[None, :])).astype(jnp.float32)
    imp = jnp.einsum('bghsn,nj->bgsj', p_cmp, overlap)
    t = jnp.arange(s_len)[:, None]
    j = jnp.arange(n_sel)[None, :]
    cur = t // SEL_BLOCK
    valid = j * SEL_BLOCK <= t
    forced = (j == 0) | (j == cur) | (j == cur - 1)
    score = jnp.where(forced, FORCE, jnp.where(valid, imp, NEG))
    _, idx = lax.top_k(score, min(SEL_TOPN, n_sel))
    return idx


def sel_attention(q, ks, vs, idx):
    bsz, g, hpg, s_len, dh = q.shape
    n_sel = s_len // SEL_BLOCK
    k_top = idx.shape[-1]
    nc = s_len // SEL_QCHUNK
    kb = ks.reshape(bsz, g, n_sel, SEL_BLOCK, dh)
    vb = vs.reshape(bsz, g, n_sel, SEL_BLOCK, dh)
    qc = q.reshape(bsz, g, hpg, nc, SEL_QCHUNK, dh).transpose(3, 0, 1, 2, 4, 5)
    ic = idx.reshape(bsz, g, nc, SEL_QCHUNK, k_top).transpose(2, 0, 1, 3, 4)
    starts = jnp.arange(nc) * SEL_QCHUNK
    gather = jax.vmap(jax.vmap(lambda blk, ix: blk[ix]))

    def chunk(args):
        qq, ii, start = args
        kg = gather(kb, ii)
        vg = gather(vb, ii)
        sc = jnp.einsum('bghqd,bgqkrd->bghqkr', qq, kg).astype(jnp.float32) * HEAD_DIM ** -0.5
        pos = ii[..., None] * SEL_BLOCK + jnp.arange(SEL_BLOCK)
        tq = start + jnp.arange(SEL_QCHUNK)
        mask = (pos <= tq[None, None, :, None, None])[:, :, None]
        sc = jnp.where(mask, sc, NEG).reshape(bsz, g, hpg, SEL_QCHUNK, k_top * SEL_BLOCK)
        p = jax.nn.softmax(sc, axis=-1).reshape(bsz, g, hpg, SEL_QCHUNK, k_top, SEL_BLOCK)
        return jnp.einsum('bghqkr,bgqkrd->bghqd', p.astype(vg.dtype), vg)

    o = lax.map(chunk, (qc, ic, starts))
    return o.transpose(1, 2, 3, 0, 4, 5).reshape(bsz, g, hpg, s_len, dh)


def win_attention(q, kw, vw):
    bsz, g, hpg, s_len, dh = q.shape
    nb = s_len // WIN_QBLOCK
    span = WINDOW // WIN_QBLOCK
    ksz = (span + 1) * WIN_QBLOCK
    padw = ((0, 0), (0, 0), (WINDOW, 0), (0, 0))
    kp = jnp.pad(kw, padw).reshape(bsz, g, nb + span, WIN_QBLOCK, dh)
    vp = jnp.pad(vw, padw).reshape(bsz, g, nb + span, WIN_QBLOCK, dh)
    kband = jnp.concatenate([kp[:, :, i:i + nb] for i in range(span + 1)], axis=3)
    vband = jnp.concatenate([vp[:, :, i:i + nb] for i in range(span + 1)], axis=3)
    qb = q.reshape(bsz, g, hpg, nb, WIN_QBLOCK, dh)
    sc = jnp.einsum('bghnqd,bgnkd->bghnqk', qb, kband).astype(jnp.float32) * HEAD_DIM ** -0.5
    blk = jnp.arange(nb)[:, None, None] * WIN_QBLOCK
    tq = blk + jnp.arange(WIN_QBLOCK)[None, :, None]
    sk = blk - WINDOW + jnp.arange(ksz)[None, None, :]
    mask = (sk <= tq) & (tq - sk < WINDOW) & (sk >= 0)
    p = jax.nn.softmax(jnp.where(mask, sc, NEG), axis=-1)
    o = jnp.einsum('bghnqk,bgnkd->bghnqd', p.astype(vband.dtype), vband)
    return o.reshape(bsz, g, hpg, s_len, dh)


def short_conv(u, b_gate, c_gate, w):
    s_len = u.shape[1]
    z = jnp.pad(c_gate * u, ((0, 0), (CONV_W - 1, 0), (0, 0)))
    y = z[:, 0:s_len] * w[0]
    for i in range(1, CONV_W):
        y = y + z[:, i:i + s_len] * w[i]
    return b_gate * y


def hybrid_mixer(h, w_in, cmp_pos, cmp_w1, cmp_b1, cmp_w2, cmp_b2, conv_w, w_o):
    bsz, s_len, _ = h.shape
    z = h @ w_in
    offsets = np.cumsum(SPLIT_WIDTHS)[:-1].tolist()
    q, kc, vc, ks, vs, kw, vw, gates, u, bg, cg = jnp.split(z, offsets, axis=-1)
    q = q.reshape(bsz, s_len, N_KV, HPG, HEAD_DIM).transpose(0, 2, 3, 1, 4)

    def kv(t):
        return t.reshape(bsz, s_len, N_KV, HEAD_DIM).transpose(0, 2, 1, 3)

    kc_c = compress(kv(kc), cmp_pos[0], cmp_w1[0], cmp_b1[0], cmp_w2[0], cmp_b2[0])
    vc_c = compress(kv(vc), cmp_pos[1], cmp_w1[1], cmp_b1[1], cmp_w2[1], cmp_b2[1])
    o_cmp, p_cmp = cmp_attention(q, kc_c, vc_c)
    idx = select_blocks(p_cmp, s_len)
    o_sel = sel_attention(q, kv(ks), kv(vs), idx)
    o_win = win_attention(q, kv(kw), kv(vw))
    g = jax.nn.sigmoid(gates).reshape(bsz, s_len, 3, N_KV, HPG).transpose(2, 0, 3, 4, 1)[..., None]
    o_attn = g[0] * o_cmp + g[1] * o_sel + g[2] * o_win
    o_attn = o_attn.transpose(0, 3, 1, 2, 4).reshape(bsz, s_len, ATTN_W)
    o_conv = short_conv(u, bg, cg, conv_w)
    return jnp.concatenate([o_attn, o_conv], axis=-1) @ w_o


def swiglu(h, wg, wu, wd):
    return (jax.nn.silu(h @ wg) * (h @ wu)) @ wd


def moe_swiglu(h, w_router, wg, wu, wd):
    logits = (h @ w_router).astype(jnp.float32)
    top_v, top_i = lax.top_k(logits, TOP_K)
    w_top = jax.nn.softmax(top_v, axis=-1)
    gate = jnp.sum(jax.nn.one_hot(top_i, N_EXPERTS, dtype=jnp.float32) * w_top[..., None], axis=-2)
    out = jnp.zeros_like(h)
    for e in range(N_EXPERTS):
        out = out + gate[..., e:e + 1].astype(h.dtype) * swiglu(h, wg[e], wu[e], wd[e])
    return out


def setup_inputs(seed: int = 0) -> dict:
    key = jax.random.key(seed)
    ks = jax.random.split(key, 24)

    def nrm(k, shape, scale):
        return jax.random.normal(k, shape, jnp.float32) * scale

    return {
        'x': nrm(ks[0], (BATCH, SEQ, D_MODEL), 1.0),
        'ln_in_g': 1.0 + nrm(ks[1], (D_MODEL,), 0.02),
        'ln_in_b': nrm(ks[2], (D_MODEL,), 0.02),
        'w_in': nrm(ks[3], (DEPTH, D_MODEL, D_IN), D_MODEL ** -0.5),
        'cmp_pos': nrm(ks[4], (DEPTH, 2, CMP_BLOCK, HEAD_DIM), 0.1),
        'cmp_w1': nrm(ks[5], (DEPTH, 2, CMP_BLOCK * HEAD_DIM, CMP_HIDDEN), (CMP_BLOCK * HEAD_DIM) ** -0.5),
        'cmp_b1': nrm(ks[6], (DEPTH, 2, CMP_HIDDEN), 0.02),
        'cmp_w2': nrm(ks[7], (DEPTH, 2, CMP_HIDDEN, HEAD_DIM), CMP_HIDDEN ** -0.5),
        'cmp_b2': nrm(ks[8], (DEPTH, 2, HEAD_DIM), 0.02),
        'conv_w': nrm(ks[9], (DEPTH, CONV_W, CONV_CH), CONV_W ** -0.5),
        'w_o': nrm(ks[10], (DEPTH, D_MIX, D_MODEL), BETA * D_MIX ** -0.5),
        'ln1_g': 1.0 + nrm(ks[11], (DEPTH, D_MODEL), 0.02),
        'ln1_b': nrm(ks[12], (DEPTH, D_MODEL), 0.02),
        'ln2_g': 1.0 + nrm(ks[13], (DEPTH, D_MODEL), 0.02),
        'ln2_b': nrm(ks[14], (DEPTH, D_MODEL), 0.02),
        'ffn_wg': nrm(ks[15], (N_DENSE, D_MODEL, D_FF), D_MODEL ** -0.5),
        'ffn_wu': nrm(ks[16], (N_DENSE, D_MODEL, D_FF), D_MODEL ** -0.5),
        'ffn_wd': nrm(ks[17], (N_DENSE, D_FF, D_MODEL), BETA * D_FF ** -0.5),
        'moe_router': nrm(ks[18], (N_MOE, D_MODEL, N_EXPERTS), D_MODEL ** -0.5),
        'moe_wg': nrm(ks[19], (N_MOE, N_EXPERTS, D_MODEL, D_FF_EXPERT), D_MODEL ** -0.5),
        'moe_wu': nrm(ks[20], (N_MOE, N_EXPERTS, D_MODEL, D_FF_EXPERT), D_MODEL ** -0.5),
        'moe_wd': nrm(ks[21], (N_MOE, N_EXPERTS, D_FF_EXPERT, D_MODEL), BETA * D_FF_EXPERT ** -0.5),
    }


def reference(x, ln_in_g, ln_in_b, w_in, cmp_pos, cmp_w1, cmp_b1, cmp_w2, cmp_b2, conv_w, w_o,
              ln1_g, ln1_b, ln2_g, ln2_b, ffn_wg, ffn_wu, ffn_wd,
              moe_router, moe_wg, moe_wu, moe_wd):
    h = layer_norm(x, ln_in_g, ln_in_b)
    for l in range(DEPTH):
        m = hybrid_mixer(h, w_in[l], cmp_pos[l], cmp_w1[l], cmp_b1[l], cmp_w2[l], cmp_b2[l],
                         conv_w[l], w_o[l])
        h = layer_norm(ALPHA * h + m, ln1_g[l], ln1_b[l])
        if l % 2 == 0:
            f = swiglu(h, ffn_wg[l // 2], ffn_wu[l // 2], ffn_wd[l // 2])
        else:
            f = moe_swiglu(h, moe_router[l // 2], moe_wg[l // 2], moe_wu[l // 2], moe_wd[l // 2])
        h = layer_norm(ALPHA * h + f, ln2_g[l], ln2_b[l])
    return h
```

```python
import numpy as np
from contextlib import ExitStack
import concourse.bass as bass
import concourse.mybir as mybir
from concourse.bass_utils import run_bass_kernel_spmd

F32 = mybir.dt.float32
BF16 = mybir.dt.bfloat16
AF = mybir.ActivationFunctionType
ALU = mybir.AluOpType

S = 2048
D = 1024
NT = 16
KC = 8
DEPTH = 2
D_IN = 2840
D_FF = 2816
NE = 8
D_FFE = 1408
ALPHA = float((2 * DEPTH) ** 0.25)
LN_EPS = 1e-5
BIG = 16384.0
FORCE = 1e9
NEG = -1e30

C_Q, C_KC, C_VC, C_KS, C_VS, C_KW, C_VW, C_G, C_U, C_B, C_C = 0, 512, 640, 768, 896, 1024, 1152, 1280, 1304, 1816, 2328

EPOCH = 20000
NDMASEM = 12


import types as _types


def _freeze(fn):
    if fn.__closure__ is None:
        return fn
    cells = []
    for c in fn.__closure__:
        try:
            cells.append(_types.CellType(c.cell_contents))
        except ValueError:
            cells.append(c)
    g = _types.FunctionType(fn.__code__, fn.__globals__, fn.__name__, fn.__defaults__, tuple(cells))
    g.__kwdefaults__ = fn.__kwdefaults__
    return g


class Prog:
    def __init__(self, nc):
        self.nc = nc
        self.ops = []
        self.engs = {'pe': nc.tensor, 'act': nc.scalar, 'dve': nc.vector, 'pool': nc.gpsimd, 'sp': nc.sync}
        self.known = set()
        self.last_release = None

    def op(self, eng, fn, reads=(), writes=(), dma=False):
        if _DIS[0]:
            return None
        banks = []
        for k in tuple(reads) + tuple(writes):
            b = k if isinstance(k, str) else k[0]
            if isinstance(b, str) and b.startswith('ps'):
                bk = ('BANK', 'psX' if b.startswith('psX') else b)
                if bk not in banks:
                    banks.append(bk)
        writes = tuple(writes) + tuple(banks)
        extra = []
        for k in tuple(reads) + tuple(writes):
            if k not in self.known:
                self.known.add(k)
                if self.last_release is not None:
                    extra.append(('init', k, self.last_release))
        self.ops.append([eng, _freeze(fn), tuple(reads), tuple(writes), dma, extra])
        return len(self.ops) - 1

    def dma(self, queue, fn, reads=(), writes=()):
        return self.op(queue, fn, reads, writes, dma=True)

    def release(self, keys, scratch):
        if _DIS[0]:
            return
        nc = self.nc
        keys = [k for k in keys if k in self.known]
        i = self.op('dve', lambda: nc.vector.memset(scratch, 0.0), reads=(), writes=tuple(keys) + ('__rel__',))
        for k in keys:
            self.known.discard(k)
        self.last_release = i

    def emit(self):
        nc = self.nc
        ops = self.ops
        n = len(ops)
        last_writer = {}
        readers = {}
        deps = [None] * n
        needed = [False] * n
        for i, (eng, fn, reads, writes, dma, extra) in enumerate(ops):
            for (_, k, rel) in extra:
                if last_writer.get(k, -1) < rel:
                    last_writer[k] = rel
                    readers[k] = []
            d = set()
            for r in reads:
                w = last_writer.get(r)
                if w is not None:
                    d.add(w)
            for w_ in writes:
                w = last_writer.get(w_)
                if w is not None:
                    d.add(w)
                for rd in readers.get(w_, ()):
                    d.add(rd)
            dd = []
            for j in d:
                if j == i:
                    continue
                ej, dj = ops[j][0], ops[j][4]
                if not dj and not dma and ej == eng:
                    if eng == 'pe':
                        continue
                dd.append(j)
            deps[i] = dd
            for j in dd:
                needed[j] = True
            for w_ in writes:
                last_writer[w_] = i
                readers[w_] = []
            for r in reads:
                readers.setdefault(r, []).append(i)
        eng_sems = {}
        counts = {e: 0 for e in self.engs}
        dma_sems = {}
        dma_counts = {}
        signal = [None] * n
        waited = {e: {} for e in self.engs}

        def get_eng_sem(e, epoch):
            key = (e, epoch)
            if key not in eng_sems:
                eng_sems[key] = nc.alloc_semaphore(name=f"s_{e}_{epoch}")
            return eng_sems[key]

        def get_dma_sem(q, k):
            key = (q, k)
            if key not in dma_sems:
                dma_sems[key] = nc.alloc_semaphore(name=f"d_{q}_{k}")
            return dma_sems[key]

        def do_wait(e, sem, val):
            sid = id(sem)
            if waited[e].get(sid, 0) >= val:
                return
            waited[e][sid] = val
            self.engs[e].wait_ge(sem, val)

        for i, (eng, fn, reads, writes, dma, extra) in enumerate(ops):
            for j in deps[i]:
                s = signal[j]
                assert s is not None, (i, j)
                do_wait(eng, s[0], s[1])
            if dma:
                k = dma_counts.get(eng, 0)
                dma_counts[eng] = k + 1
                sem = get_dma_sem(eng, k % NDMASEM)
                val = 16 * (k // NDMASEM + 1)
                if val > 16:
                    do_wait(eng, sem, val - 16)
                inst = fn()
                inst.then_inc(sem, 16)
                signal[i] = (sem, val)
            else:
                inst = fn()
                if needed[i]:
                    c = counts[eng]
                    counts[eng] = c + 1
                    sem = get_eng_sem(eng, c // EPOCH)
                    val = c % EPOCH + 1
                    inst.then_inc(sem, 1)
                    signal[i] = (sem, val)
        self.signal = signal
        last = {}
        for s in signal:
            if s is not None:
                sid = id(s[0])
                if sid not in last or last[sid][1] < s[1]:
                    last[sid] = s
        for sem, val in last.values():
            self.engs['sp'].wait_ge(sem, val)


class _Stop(Exception):
    pass


import os as _os


_DIS = [False]


def chk(tag):
    if _os.environ.get("KSTOP") == tag:
        _DIS[0] = True


def _units(total_chunks, per):
    out = []
    c = 0
    while c < total_chunks:
        n = min(per, total_chunks - c)
        out.append((c, n))
        c += n
    return out


def build_nc(upto="all", dbg=()):
    nc = bass.Bass("TRN2", target_bir_lowering=False)
    P = Prog(nc)

    def din(name, shape, dt=F32):
        return nc.dram_tensor(name, list(shape), dt, kind="ExternalInput").ap()

    x_d = din("x", [S, D])
    w_in_d = din("w_in", [DEPTH, D, D_IN])
    w1_d = din("cmp_w1", [DEPTH, 2, 2048, 256])
    w2_d = din("cmp_w2", [DEPTH, 2, 256, 64])
    w_o_d = din("w_o", [DEPTH, D, D])
    wg_d = din("ffn_wg", [1, D, D_FF])
    wu_d = din("ffn_wu", [1, D, D_FF])
    wd_d = din("ffn_wd", [1, D_FF, D])
    wr_d = din("moe_router", [1, D, NE])
    mwg_d = din("moe_wg", [1, NE, D, D_FFE])
    mwu_d = din("moe_wu", [1, NE, D, D_FFE])
    mwd_d = din("moe_wd", [1, NE, D_FFE, D])
    lnp_d = din("lnp", [10, D])
    posT_d = din("posT", [DEPTH, 2, 64, 32])
    b1T_d = din("b1T", [DEPTH, 2, 128, 2])
    b2c_d = din("b2c", [DEPTH, 2, 64, 1])
    b2r_d = din("b2r", [DEPTH, 64])
    convT_d = din("convT", [DEPTH, 128, 4, 3])
    ident_d = din("ident", [128, 128])
    cmpmask_d = din("cmpmask", [128, S])
    tric_d = din("tric", [128, 128])
    trib_d = din("trib", [128, 128])
    emat_d = din("emat", [32, S])
    selb_d = din("selb", [128, NT, 32])
    selv_d = din("selv", [128, NT, 32])
    ovl_d = din("ovl", [128, 32])
    y_d = nc.dram_tensor("y", [S, D], F32, kind="ExternalOutput").ap()
    hres_d = nc.dram_tensor("hres", [S, D], F32).ap()
    dbg_out = {}

    es = ExitStack()
    with es:
        def sb(name, shape, dt=F32, stack=es):
            return stack.enter_context(nc.sbuf_tensor("sb_" + name, list(shape), dt))

        def ps(name, shape, dt=F32):
            return es.enter_context(nc.psum_tensor(name, list(shape), dt))

        psS = [ps(f"psS{i}", [128, 512]) for i in range(2)]
        psU = [ps(f"psU{i}", [128, 512]) for i in range(2)]
        psO = [ps(f"psO{i}", [128, 512]) for i in range(2)]
        psT = ps("psT", [128, 1024], BF16)
        psX = ps("psX", [128, 512])

        ident = sb("identb", [128, 128], BF16)
        hT = sb("hT", [128, KC, S], BF16)
        lnG = sb("lnG", [128, D])
        lnB = sb("lnB", [128, D])
        cmpmask = sb("cmpmask", [128, S], BF16)
        tric = sb("tric", [128, 128], BF16)
        trib = sb("trib", [128, 128], BF16)
        selb = sb("selb", [128, NT, 32])
        selv = sb("selv", [128, NT, 32])
        scratch = sb("scratch", [128, 8])

        P.dma('pool', lambda: nc.gpsimd.dma_start(out=ident[:], in_=ident_d), writes=['ident'])
        P.dma('pool', lambda: nc.gpsimd.dma_start(out=cmpmask[:], in_=cmpmask_d), writes=['cmpmask'])
        P.dma('pool', lambda: nc.gpsimd.dma_start(out=tric[:], in_=tric_d), writes=['tric'])
        P.dma('pool', lambda: nc.gpsimd.dma_start(out=trib[:], in_=trib_d), writes=['trib'])
        P.dma('sp', lambda: nc.sync.dma_start(out=selb[:], in_=selb_d), writes=['selb'])
        P.dma('sp', lambda: nc.sync.dma_start(out=selv[:], in_=selv_d), writes=['selv'])

        lnctr = [0]

        def load_ln(idx):
            P.dma('sp', lambda: nc.sync.dma_start(out=lnG[:], in_=lnp_d[2 * idx, :].partition_broadcast(128)), writes=['lnG'])
            P.dma('sp', lambda: nc.sync.dma_start(out=lnB[:], in_=lnp_d[2 * idx + 1, :].partition_broadcast(128)), writes=['lnB'])

        mvall = [sb(f"mvall{i}", [128, 4, 8]) for i in range(2)]
        stall = [sb(f"stall{i}", [128, 4, 2, 6]) for i in range(2)]
        hb3 = [sb(f"hb3_{i}", [128, D], BF16) for i in range(3)]
        lngctr = [0]
        hbctr = [0]

        def ln_stats(tiles):
            b = lngctr[0] % 2
            lngctr[0] += 1
            n = len(tiles)
            m, st = mvall[b], stall[b]
            for k, (xt, xkey, tt) in enumerate(tiles):
                for c in range(2):
                    P.op('dve', lambda c=c, k=k, xt=xt: nc.vector.bn_stats(out=st[:, k, c, :], in_=xt[:, c * 512:(c + 1) * 512]), reads=[xkey], writes=[('lst', b, k, c)])
                P.op('dve', lambda k=k: nc.vector.bn_aggr(out=m[:, k, 0:2], in_=st[:, k, :, :]), reads=[('lst', b, k, 0), ('lst', b, k, 1)], writes=[('lmv', b, k)])
            allmv = [('lmv', b, k) for k in range(n)]
            P.op('dve', lambda: nc.vector.tensor_scalar(out=m[:, 0:n, 2], in0=m[:, 0:n, 1], scalar1=LN_EPS, scalar2=None, op0=ALU.add), reads=allmv, writes=[('lmvb', b)])
            P.op('act', lambda: nc.scalar.activation(out=m[:, 0:n, 3], in_=m[:, 0:n, 2], func=AF.Sqrt), reads=[('lmvb', b)], writes=[('lmvc', b)])
            P.op('dve', lambda: nc.vector.reciprocal(out=m[:, 0:n, 4], in_=m[:, 0:n, 3]), reads=[('lmvc', b)], writes=[('lmvd', b)])
            P.op('dve', lambda: nc.vector.scalar_tensor_tensor(out=m[:, 0:n, 5], in0=m[:, 0:n, 0], scalar=-1.0, in1=m[:, 0:n, 4], op0=ALU.mult, op1=ALU.mult), reads=allmv + [('lmvd', b)], writes=[('lmve', b)])
            return b

        def ln_apply(tiles, b, dst):
            m = mvall[b]
            hbs = []

            def stage_a(k):
                xt, xkey, tt = tiles[k]
                P.op('act', lambda: nc.scalar.activation(out=xt, in_=xt, func=AF.Identity, bias=m[:, k, 5:6], scale=m[:, k, 4:5]), reads=[xkey, ('lmvd', b), ('lmve', b)], writes=[xkey])
                P.op('dve', lambda: nc.vector.tensor_tensor(out=xt, in0=xt, in1=lnG[:], op=ALU.mult), reads=[xkey, 'lnG'], writes=[xkey])
                P.op('pool', lambda: nc.gpsimd.tensor_tensor(out=xt, in0=xt, in1=lnB[:], op=ALU.add), reads=[xkey, 'lnB'], writes=[xkey])

            def stage_b(k):
                xt, xkey, tt = tiles[k]
                hi = hbctr[0] % 3
                hbctr[0] += 1
                hbt, hk = hb3[hi], f"hb3_{hi}"
                P.op('act', lambda: nc.scalar.copy(out=hbt[:], in_=xt), reads=[xkey], writes=[hk])
                P.dma('sp', lambda: nc.sync.dma_start(out=dst[tt * 128:(tt + 1) * 128, :], in_=xt), reads=[xkey], writes=[('hres', tt)])
                for kk in range(KC):
                    P.op('pe', lambda kk=kk: nc.tensor.transpose(out=psT[:, kk * 128:(kk + 1) * 128], in_=hbt[:, kk * 128:(kk + 1) * 128], identity=ident[:]), reads=[hk, 'ident'], writes=['psT'])
                P.op('dve', lambda: nc.vector.tensor_copy(out=hT[:, :, tt * 128:(tt + 1) * 128], in_=psT[:].rearrange("p (k t) -> p k t", k=KC)), reads=['psT'], writes=[('hT', tt)])

            n = len(tiles)
            for k in range(n + 2):
                if k < n:
                    stage_a(k)
                if 0 <= k - 2 < n:
                    stage_b(k - 2)

        def ln_group(tiles, dst):
            ln_apply(tiles, ln_stats(tiles), dst)

        HT_ALL = [('hT', tt) for tt in range(NT)]

        with ExitStack() as s0:
            xt = [sb(f"xt{i}", [128, D], stack=s0) for i in range(8)]
            load_ln(0)
            grp = []
            for gi in range(5):
                if gi < 4:
                    tiles = []
                    for k in range(4):
                        tt = gi * 4 + k
                        i = (gi % 2) * 4 + k
                        P.dma('sp', lambda tt=tt, i=i: nc.sync.dma_start(out=xt[i][:], in_=x_d[tt * 128:(tt + 1) * 128, :]), writes=[f"xt{i}"])
                        tiles.append((xt[i][:], f"xt{i}", tt))
                    grp.append((tiles, ln_stats(tiles)))
                if gi >= 1:
                    ln_apply(grp[gi - 1][0], grp[gi - 1][1], hres_d if upto != "ln0" else y_d)
            P.release([f"xt{i}" for i in range(8)], scratch[:, 0:1])

        def finish():
            if _DIS[0]:
                dbg_out.clear()
                _DIS[0] = False
            for name, (tile_ap, keys, shape, dt) in dbg_out.items():
                d = nc.dram_tensor("dbg_" + name, list(shape), dt, kind="ExternalOutput").ap()
                P.dma('sp', lambda d=d, tile_ap=tile_ap: nc.sync.dma_start(out=d, in_=tile_ap), reads=keys)
            P.emit()

        if upto == "ln0":
            if 'hT' in dbg:
                dbg_out['hT'] = (hT[:], HT_ALL, [128, KC, S], BF16)
            finish()
            return nc

        n_layers = DEPTH if upto in ("all",) else (1 if not upto.startswith("L1") else 2)
        try:
            for l in range(n_layers):
                last_layer = (l == DEPTH - 1)
                stop = upto.split(":")[1] if (":" in upto and l == n_layers - 1) else None
                with ExitStack() as sm:
                    mkeys = []

                    def sbm(name, shape, dt=F32, stack=sm):
                        return stack.enter_context(nc.sbuf_tensor(f"sb_{name}_{l}", list(shape), dt))

                    mixT = sbm("mixT", [128, KC, S], BF16)
                    gates = sbm("gates", [128, NT, 24])
                    vs_aug = sbm("vs_aug", [128, NT, 2, 65], BF16)
                    vw_aug = sbm("vw_aug", [128, NT, 2, 65], BF16)
                    wst = [sbm(f"wst{i}", [128, KC, 384], BF16) for i in range(2)]
                    kccT = sbm("kccT", [128, 2, 128], BF16)
                    vcc = sbm("vcc", [128, 2, 97], BF16)
                    convw = sbm("convw", [128, 4, 3])
                    MIXT = [('mixT', l, c, tc) for c in range(KC) for tc in range(4)]
                    wctr = [0]

                    def load_wblock(segs):
                        i = wctr[0] % 2
                        wctr[0] += 1
                        keys = [(f"wst{i}", k) for k in range(3)]
                        off = 0
                        offs = []
                        for si_, (c0, ncol) in enumerate(segs):
                            wk = [keys[si_]] if si_ < len(segs) - 1 else keys[si_:]
                            P.dma('pool', lambda c0=c0, ncol=ncol, off=off, i=i: nc.gpsimd.dma_start(
                                out=wst[i][:, :, off:off + ncol],
                                in_=w_in_d[l].rearrange("(kc p) n -> p kc n", p=128)[:, :, c0:c0 + ncol]), writes=wk)
                            offs.append(off)
                            off += ncol
                        return wst[i], keys, offs

                    pctr = [0]

                    def proj_fm(wt, wkey, off, M, evac):
                        for tc in range(4):
                            i = pctr[0] % 2
                            pctr[0] += 1
                            for kc in range(KC):
                                P.op('pe', lambda kc=kc, tc=tc, i=i: nc.tensor.matmul(
                                    psS[i][0:M, :], lhsT=wt[:, kc, off:off + M], rhs=hT[:, kc, tc * 512:(tc + 1) * 512],
                                    start=(kc == 0), stop=(kc == KC - 1)),
                                    reads=list(wkey) + HT_ALL[tc * 4:tc * 4 + 4], writes=[f"psS{i}"])
                            evac(tc, psS[i], f"psS{i}")

                    P.dma('sp', lambda: nc.sync.dma_start(out=convw[:], in_=convT_d[l]), writes=['convw'])
                    P.op('pool', lambda: nc.gpsimd.memset(vs_aug[:, :, :, 64:65], 1.0), writes=['vs_ones'])
                    P.op('pool', lambda: nc.gpsimd.memset(vw_aug[:, :, :, 64:65], 1.0), writes=['vw_ones'])

                    with ExitStack() as sc_:
                        kvT = sbm("kvT", [64, 4, S], BF16, sc_)
                        kcp = sbm("kcp", [64, 32, 128], BF16, sc_)
                        w1s = sbm("w1s", [64, 32, 256], BF16, sc_)
                        w2s = sbm("w2s", [128, 2, 64], BF16, sc_)
                        posT = sbm("posT", [64, 32], F32, sc_)
                        b1T = sbm("b1T", [128, 2], F32, sc_)
                        b2c = sbm("b2c", [64, 1], F32, sc_)
                        b2r = sbm("b2r", [128, 64], F32, sc_)
                        ovl = sbm("ovl", [128, 32], F32, sc_)
                        gx = sbm("gx", [128, 128], F32, sc_)
                        gu = sbm("gu", [128, 128], F32, sc_)
                        gs_ = sbm("gs", [128, 128], F32, sc_)
                        hact = sbm("hact", [128, 2, 128], BF16, sc_)
                        wt, wkey, offs = load_wblock([(C_KC, 256)])
                        for idx in range(4):
                            def evac(tc, pt, pk, idx=idx):
                                P.op('act', lambda: nc.scalar.copy(out=kvT[:, idx, tc * 512:(tc + 1) * 512], in_=pt[0:64, :]), reads=[pk], writes=[('kvT', idx, tc)])
                            proj_fm(wt, wkey, offs[0] + idx * 64, 64, evac)
                        chk('A')
                        P.op('pool', lambda: nc.gpsimd.memset(vcc[:], 0.0), writes=['vcc'])
                        P.op('pool', lambda: nc.gpsimd.memset(kccT[:], 0.0), writes=['kccT'])
                        P.op('pool', lambda: nc.gpsimd.memset(vcc[:, :, 64:65], 1.0), reads=[], writes=['vcc'])
                        P.dma('sp', lambda: nc.sync.dma_start(out=ovl[:], in_=ovl_d), writes=['ovl'])
                        P.op('act', lambda: nc.scalar.copy(out=vcc[:, 0, 65:97], in_=ovl[:]), reads=['ovl'], writes=['vcc'])
                        P.op('act', lambda: nc.scalar.copy(out=vcc[:, 1, 65:97], in_=ovl[:]), reads=['ovl'], writes=['vcc'])
                        P.dma('sp', lambda: nc.sync.dma_start(out=b2r[:], in_=b2r_d[l, :].partition_broadcast(128)), writes=['b2r'])
                        chk('B')
                        for kv in range(2):
                            for r4 in range(4):
                                P.dma('pool', lambda kv=kv, r4=r4: nc.gpsimd.dma_start(out=w1s[:, r4 * 8:(r4 + 1) * 8, :], in_=w1_d[l, kv].rearrange("(r d) n -> d r n", d=64)[:, r4 * 8:(r4 + 1) * 8, :]), writes=[('w1s', r4)])
                            P.dma('pool', lambda kv=kv: nc.gpsimd.dma_start(out=w2s[:], in_=w2_d[l, kv].rearrange("(c p) n -> p c n", p=128)), writes=['w2s'])
                            P.dma('sp', lambda kv=kv: nc.sync.dma_start(out=posT[:], in_=posT_d[l, kv]), writes=['posT'])
                            P.dma('sp', lambda kv=kv: nc.sync.dma_start(out=b1T[:], in_=b1T_d[l, kv]), writes=['b1T'])
                            P.dma('sp', lambda kv=kv: nc.sync.dma_start(out=b2c[:], in_=b2c_d[l, kv]), writes=['b2c'])
                            chk('C')
                            for g in range(2):
                                idx = kv * 2 + g
                                for r in range(32):
                                    P.op('dve', lambda r=r, idx=idx: nc.vector.tensor_scalar(
                                        out=kcp[:, r, 0:127], in0=kvT[:, idx, r:r + 16 * 126 + 1:16], scalar1=posT[:, r:r + 1], scalar2=None, op0=ALU.add),
                                        reads=[('kvT', idx, tc) for tc in range(4)] + ['posT'], writes=[('kcp', r)])
                                chk('D')
                                for hc in range(2):
                                    for r in range(32):
                                        P.op('pe', lambda r=r, hc=hc: nc.tensor.matmul(
                                            psX[:, 0:127], lhsT=w1s[:, r, hc * 128:(hc + 1) * 128], rhs=kcp[:, r, 0:127], start=(r == 0), stop=(r == 31)),
                                            reads=[('w1s', r // 8), ('kcp', r)], writes=['psX'])
                                    P.op('act', lambda hc=hc: nc.scalar.activation(out=gx[:, 0:127], in_=psX[:, 0:127], func=AF.Identity, bias=b1T[:, hc:hc + 1], scale=1.0), reads=['psX', 'b1T'], writes=['gx'])
                                    P.op('dve', lambda: nc.vector.tensor_tensor(out=gu[:, 0:127], in0=gx[:, 0:127], in1=gx[:, 0:127], op=ALU.mult), reads=['gx'], writes=['gu'])
                                    P.op('dve', lambda: nc.vector.tensor_scalar(out=gu[:, 0:127], in0=gu[:, 0:127], scalar1=0.044715, scalar2=1.0, op0=ALU.mult, op1=ALU.add), reads=['gu'], writes=['gu'])
                                    P.op('dve', lambda: nc.vector.tensor_tensor(out=gu[:, 0:127], in0=gu[:, 0:127], in1=gx[:, 0:127], op=ALU.mult), reads=['gu', 'gx'], writes=['gu'])
                                    P.op('act', lambda: nc.scalar.activation(out=gs_[:, 0:127], in_=gu[:, 0:127], func=AF.Sigmoid, scale=1.5957691216057308), reads=['gu'], writes=['gs'])
                                    P.op('dve', lambda hc=hc: nc.vector.tensor_tensor(out=hact[:, hc, 0:127], in0=gx[:, 0:127], in1=gs_[:, 0:127], op=ALU.mult), reads=['gx', 'gs'], writes=[('hact', hc)])
                                    chk('E')
                                chk('E2')
                                if kv == 0:
                                    for hc in range(2):
                                        P.op('pe', lambda hc=hc: nc.tensor.matmul(psX[0:64, 128:255], lhsT=w2s[:, hc, :], rhs=hact[:, hc, 0:127], start=(hc == 0), stop=(hc == 1)),
                                             reads=['w2s', ('hact', hc)], writes=['psXb'])
                                    chk('F1')
                                    P.op('dve', lambda g=g: nc.vector.tensor_scalar(out=kccT[0:64, g, 0:127], in0=psX[0:64, 128:255], scalar1=b2c[:, 0:1], scalar2=None, op0=ALU.add), reads=['psXb', 'b2c'], writes=['kccT'])
                                    chk('F')
                                else:
                                    for hc in range(2):
                                        P.op('pe', lambda hc=hc: nc.tensor.matmul(psX[0:127, 256:320], lhsT=hact[:, hc, 0:127], rhs=w2s[:, hc, :], start=(hc == 0), stop=(hc == 1)),
                                             reads=['w2s', ('hact', hc)], writes=['psXc'])
                                    P.op('dve', lambda g=g: nc.vector.tensor_tensor(out=vcc[0:127, g, 0:64], in0=psX[0:127, 256:320], in1=b2r[0:127, :], op=ALU.add), reads=['psXc', 'b2r'], writes=['vcc'])
                        if 'kvT' in dbg and stop == "cmp":
                            dbg_out['kvT'] = (kvT[:], [('kvT', i, tc) for i in range(4) for tc in range(4)], [64, 4, S], BF16)
                        if stop == "cmp":
                            dbg_out['kccT'] = (kccT[0:64, :, :], ['kccT'], [64, 2, 128], BF16)
                            dbg_out['vcc'] = (vcc[:], ['vcc'], [128, 2, 97], BF16)
                            finish()
                            return nc
                        P.release([('kvT', i, tc) for i in range(4) for tc in range(4)] + [('kcp', r) for r in range(32)] +
                                  [('w1s', 0), ('w1s', 1), ('w1s', 2), ('w1s', 3), 'w2s', 'posT', 'b1T', 'b2c', 'b2r', 'ovl', 'gx', 'gu', 'gs', ('hact', 0), ('hact', 1), 'psX', 'psXb', 'psXc'], scratch[:, 1:2])

                    with ExitStack() as sv:
                        zt = sbm("zt", [128, S + 2], F32, sv)
                        usb = [sbm(f"usb{i}", [128, 512], F32, sv) for i in range(2)]
                        yt = [sbm(f"yt{i}", [128, 512], F32, sv) for i in range(2)]
                        P.op('pool', lambda: nc.gpsimd.memset(zt[:, 0:2], 0.0), writes=['zt_halo'])
                        cctr = 0
                        for c in range(4):
                            wt, wkey, offs = load_wblock([(C_U + c * 128, 128), (C_B + c * 128, 128), (C_C + c * 128, 128)])
                            for tc in range(4):
                                i = cctr % 2
                                cctr += 1
                                for (pt, pk, off) in ((psS[i], f"psS{i}", offs[0]), (psU[i], f"psU{i}", offs[1]), (psO[i], f"psO{i}", offs[2])):
                                    for kc in range(KC):
                                        P.op('pe', lambda kc=kc, tc=tc, pt=pt, off=off: nc.tensor.matmul(
                                            pt[:, :], lhsT=wt[:, kc, off:off + 128], rhs=hT[:, kc, tc * 512:(tc + 1) * 512],
                                            start=(kc == 0), stop=(kc == KC - 1)),
                                            reads=list(wkey) + HT_ALL[tc * 4:tc * 4 + 4], writes=[pk])
                                zk = ('zt', tc)
                                zprev = [('zt', tc - 1)] if tc > 0 else ['zt_halo']
                                P.op('act', lambda i=i: nc.scalar.copy(out=usb[i][:], in_=psS[i][:]), reads=[f"psS{i}"], writes=[f"usb{i}"])
                                P.op('dve', lambda i=i, tc=tc: nc.vector.tensor_tensor(out=zt[:, 2 + tc * 512:2 + (tc + 1) * 512], in0=usb[i][:], in1=psO[i][:], op=ALU.mult),
                                     reads=[f"usb{i}", f"psO{i}"], writes=[zk])
                                P.op('dve', lambda i=i, tc=tc, c=c: nc.vector.tensor_scalar(out=yt[i][:], in0=zt[:, 2 + tc * 512:2 + (tc + 1) * 512], scalar1=convw[:, c, 2:3], scalar2=None, op0=ALU.mult),
                                     reads=[zk, 'convw'], writes=[f"yt{i}"])
                                P.op('dve', lambda i=i, tc=tc, c=c: nc.vector.scalar_tensor_tensor(out=yt[i][:], in0=zt[:, 1 + tc * 512:1 + (tc + 1) * 512], scalar=convw[:, c, 1:2], in1=yt[i][:], op0=ALU.mult, op1=ALU.add),
                                     reads=[zk, 'convw', f"yt{i}"] + zprev, writes=[f"yt{i}"])
                                P.op('dve', lambda i=i, tc=tc, c=c: nc.vector.scalar_tensor_tensor(out=yt[i][:], in0=zt[:, tc * 512:(tc + 1) * 512], scalar=convw[:, c, 0:1], in1=yt[i][:], op0=ALU.mult, op1=ALU.add),
                                     reads=[zk, 'convw', f"yt{i}"] + zprev, writes=[f"yt{i}"])
                                P.op('dve', lambda i=i, tc=tc, c=c: nc.vector.tensor_tensor(out=mixT[:, 4 + c, tc * 512:(tc + 1) * 512], in0=yt[i][:], in1=psU[i][:], op=ALU.mult),
                                     reads=[f"yt{i}", f"psU{i}"], writes=[('mixT', l, 4 + c, tc)])
                        P.release([('zt', tc) for tc in range(4)] + ['zt_halo', 'usb0', 'usb1', 'yt0', 'yt1'], scratch[:, 2:3])
                    if stop == "conv":
                        dbg_out['mixT'] = (mixT[:], MIXT[16:], [128, KC, S], BF16)
                        finish()
                        return nc

                    wt, wkey, offs = load_wblock([(C_VS, 128), (C_VW, 128), (C_G, 24)])
                    for tt in range(NT):
                        i = tt % 2
                        for kc in range(KC):
                            P.op('pe', lambda kc=kc, tt=tt, i=i: nc.tensor.matmul(
                                psS[i][:, 0:280], lhsT=hT[:, kc, tt * 128:(tt + 1) * 128], rhs=wt[:, kc, 0:280], start=(kc == 0), stop=(kc == KC - 1)),
                                reads=list(wkey) + [('hT', tt)], writes=[f"psS{i}"])
                        P.op('act', lambda tt=tt, i=i: nc.scalar.copy(out=vs_aug[:, tt, :, 0:64], in_=psS[i][:, 0:128].rearrange("p (g d) -> p g d", g=2)), reads=[f"psS{i}"], writes=[('vs', tt)])
                        P.op('dve', lambda tt=tt, i=i: nc.vector.tensor_copy(out=vw_aug[:, tt, :, 0:64], in_=psS[i][:, 128:256].rearrange("p (g d) -> p g d", g=2)), reads=[f"psS{i}"], writes=[('vw', tt)])
                        P.op('act', lambda tt=tt, i=i: nc.scalar.activation(out=gates[:, tt, :], in_=psS[i][:, 256:280], func=AF.Sigmoid), reads=[f"psS{i}"], writes=[('gates', tt)])

                    wo = sbm("wo", [128, KC, D], BF16)
                    gblocks = [load_wblock([(C_Q + 256 * g_, 256), (C_KS + 64 * g_, 64), (C_KW + 64 * g_, 64)]) for g_ in range(2)]
                    P.dma('pool', lambda: nc.gpsimd.dma_start(out=wo[:, :, 0:512], in_=w_o_d[l].rearrange("(kc p) n -> p kc n", p=128)[:, :, 0:512]), writes=[('wo', 0)])
                    P.dma('pool', lambda: nc.gpsimd.dma_start(out=wo[:, :, 512:1024], in_=w_o_d[l].rearrange("(kc p) n -> p kc n", p=128)[:, :, 512:1024]), writes=[('wo', 1)])
                    for g in range(2):
                        with ExitStack() as sg:
                            q_aug = sbm(f"q_aug{g}", [128, 4, S], BF16, sg)
                            ks_aug = sbm(f"ks_aug{g}", [128, S], BF16, sg)
                            kwT = sbm(f"kwT{g}", [128, S], BF16, sg)
                            NPT = 6
                            PT = [sbm(f"PT{g}_{i}", [128, 512], BF16, sg) for i in range(NPT)]
                            ost = [[sbm(f"ost{g}_{par}_{br}", [128, 4, 4, 97], F32, sg) for br in range(3)] for par in range(2)]
                            imp = sbm(f"imp{g}", [128, 4, 32], F32, sg)
                            itmp = sbm(f"itmp{g}", [128, 4, 32], F32, sg)
                            rden = [sbm(f"rden{g}_{par}", [128, 3, 4, 4], F32, sg) for par in range(2)]
                            sc1 = sbm(f"sc1{g}", [128, 4, 32], F32, sg)
                            sc2 = sbm(f"sc2{g}", [128, 4, 32], F32, sg)
                            m8 = sbm(f"m8{g}", [128, 4, 16], F32, sg)
                            negb = sbm(f"negb{g}", [128, 4, 96], BF16, sg)
                            oacc = sbm(f"oacc{g}", [128, 4, 256], F32, sg)
                            otmp = sbm(f"otmp{g}", [128, 4, 256], F32, sg)
                            ob = sbm(f"ob{g}", [128, 4, 256], BF16, sg)
                            gk = lambda name, *a: (name, l, g) + a
                            P.op('pool', lambda: nc.gpsimd.memset(ks_aug[64:128, :], 0.0), writes=[gk('ksE')])
                            P.op('pool', lambda: nc.gpsimd.memset(kwT[64:128, :], 0.0), writes=[gk('kwpad')])
                            P.op('pool', lambda: nc.gpsimd.memset(q_aug[64:128, :, :], 0.0), writes=[gk('qsel', hh, tc) for hh in range(4) for tc in range(4)])
                            P.dma('pool', lambda: nc.gpsimd.dma_start(out=ks_aug[64:96, :], in_=emat_d), writes=[gk('ksE')])
                            P.op('pool', lambda: nc.gpsimd.memset(negb[:, :, 0:64], 0.0), writes=[gk('negb0')])
                            wt, wkey, offs = gblocks[g]
                            for hh in range(4):
                                def evac(tc, pt, pk, hh=hh):
                                    if tc % 2 == 0:
                                        P.op('act', lambda: nc.scalar.copy(out=q_aug[0:64, hh, tc * 512:(tc + 1) * 512], in_=pt[0:64, :]), reads=[pk], writes=[gk('q', hh, tc)])
                                    else:
                                        P.op('dve', lambda: nc.vector.tensor_copy(out=q_aug[0:64, hh, tc * 512:(tc + 1) * 512], in_=pt[0:64, :]), reads=[pk], writes=[gk('q', hh, tc)])
                                proj_fm(wt, wkey, offs[0] + hh * 64, 64, evac)

                            def evac_ks(tc, pt, pk):
                                P.op('act', lambda: nc.scalar.copy(out=ks_aug[0:64, tc * 512:(tc + 1) * 512], in_=pt[0:64, :]), reads=[pk], writes=[gk('ks', tc)])
                            proj_fm(wt, wkey, offs[1], 64, evac_ks)

                            def evac_kw(tc, pt, pk):
                                P.op('act', lambda: nc.scalar.copy(out=kwT[0:64, tc * 512:(tc + 1) * 512], in_=pt[0:64, :]), reads=[pk], writes=[gk('kw', tc)])
                            proj_fm(wt, wkey, offs[2], 64, evac_kw)

                            stctr = [0]
                            ptctr = [0]
                            octr = [0]

                            def attn_items(c, br):
                                items = []
                                for hh in range(4):
                                    if br == 0:
                                        items.append((hh, 0, 0, 3, [('cmp', None)]))
                                    elif br == 1:
                                        for kt in range(4 * c + 4):
                                            i_ = kt - 4 * c
                                            jlo = max(0, i_)
                                            items.append((hh, kt, jlo, 3, [('tric', i_)] if i_ >= 0 else []))
                                    else:
                                        for kt in range(max(0, 4 * c - 4), 4 * c + 4):
                                            i_ = kt - 4 * c
                                            jlo = max(0, i_)
                                            jhi = min(3, i_ + 4)
                                            mk = []
                                            if i_ >= 0:
                                                mk.append(('tric', i_))
                                            if i_ + 4 <= 3:
                                                mk.append(('trib', i_ + 4))
                                            items.append((hh, kt, jlo, jhi, mk))
                                return items

                            def run_branch(c, br):
                                par = c % 2
                                items = attn_items(c, br)
                                first, lastk = {}, {}
                                for (hh, kt, jlo, jhi, mk) in items:
                                    for j in range(jlo, jhi + 1):
                                        first.setdefault((hh, j), kt)
                                        lastk[(hh, j)] = kt
                                nv = 97 if br == 0 else 65
                                pendq = []
                                obank = {}
                                evq = []

                                def emit_pv(item, pti):
                                    hh, kt, jlo, jhi, mk = item
                                    newbank = hh not in obank
                                    if newbank:
                                        flush_ev()
                                        obank[hh] = octr[0] % 2
                                        octr[0] += 1
                                    ob_i = obank[hh]
                                    pO = psO[ob_i]
                                    for j in range(jlo, jhi + 1):
                                        if br == 0:
                                            rhs = vcc[:, g, :]
                                            rk = ['vcc']
                                        elif br == 1:
                                            rhs = vs_aug[:, kt, g, :]
                                            rk = [('vs', kt), 'vs_ones']
                                        else:
                                            rhs = vw_aug[:, kt, g, :]
                                            rk = [('vw', kt), 'vw_ones']
                                        st_flag = newbank and (j == jlo)
                                        P.op('pe', lambda j=j, rhs=rhs, pO=pO, kt=kt, hh=hh, st_flag=st_flag: nc.tensor.matmul(
                                            pO[:, j * 97:j * 97 + nv], lhsT=PT[pti][:, j * 128:(j + 1) * 128], rhs=rhs,
                                            start=st_flag, stop=(kt == lastk[(hh, j)]), skip_group_check=True),
                                            reads=[f"PT{pti}"] + rk, writes=[f"psO{ob_i}"])
                                    if kt == max(lastk[(hh, j)] for j in range(4)):
                                        evq.append((pO, hh, ob_i))

                                def flush_ev():
                                    while evq:
                                        pO, hh, ob_i = evq.pop(0)
                                        P.op('dve', lambda pO=pO, hh=hh: nc.vector.tensor_copy(
                                            out=ost[par][br][:, hh, :, 0:nv], in_=pO[:, 0:388].rearrange("p (j n) -> p j n", j=4)[:, :, 0:nv]),
                                            reads=[f"psO{ob_i}"], writes=[gk('ost', par, br, hh)])

                                for item in items:
                                    hh, kt, jlo, jhi, mk = item
                                    si = stctr[0] % 4
                                    stctr[0] += 1
                                    pti = ptctr[0] % NPT
                                    ptctr[0] += 1
                                    c0, c1 = jlo * 128, (jhi + 1) * 128
                                    qc0 = c * 512
                                    if br == 0:
                                        lhsT, lk = kccT[:, g, :], ['kccT']
                                        rhs, rk = q_aug[:, hh, qc0 + c0:qc0 + c1], [gk('q', hh, c), gk('qsel', hh, c)]
                                    elif br == 1:
                                        lhsT, lk = ks_aug[:, kt * 128:(kt + 1) * 128], [gk('ks', kt // 4), gk('ksE')]
                                        rhs, rk = q_aug[:, hh, qc0 + c0:qc0 + c1], [gk('q', hh, c), gk('qsel', hh, c)]
                                    else:
                                        lhsT, lk = kwT[:, kt * 128:(kt + 1) * 128], [gk('kw', kt // 4), gk('kwpad')]
                                        rhs, rk = q_aug[:, hh, qc0 + c0:qc0 + c1], [gk('q', hh, c), gk('qsel', hh, c)]
                                    sbank = (psS + psU)[si]
                                    sname = ("psS0", "psS1", "psU0", "psU1")[si]
                                    P.op('pe', lambda lhsT=lhsT, rhs=rhs, sbank=sbank, c0=c0, c1=c1: nc.tensor.matmul(sbank[:, c0:c1], lhsT=lhsT, rhs=rhs, start=True, stop=True),
                                         reads=lk + rk, writes=[sname])
                                    P.op('act', lambda sbank=sbank, pti=pti, c0=c0, c1=c1: nc.scalar.activation(out=PT[pti][:, c0:c1], in_=sbank[:, c0:c1], func=AF.Exp, scale=0.125),
                                         reads=[sname], writes=[f"PT{pti}"])
                                    flush_ev()
                                    for (mname, mj) in mk:
                                        if mname == 'cmp':
                                            P.op('dve', lambda pti=pti: nc.vector.tensor_tensor(out=PT[pti][:], in0=PT[pti][:], in1=cmpmask[:, qc0:qc0 + 512], op=ALU.mult),
                                                 reads=[f"PT{pti}", 'cmpmask'], writes=[f"PT{pti}"])
                                        else:
                                            mt = tric if mname == 'tric' else trib
                                            P.op('dve', lambda pti=pti, mj=mj, mt=mt: nc.vector.tensor_tensor(out=PT[pti][:, mj * 128:(mj + 1) * 128], in0=PT[pti][:, mj * 128:(mj + 1) * 128], in1=mt[:], op=ALU.mult),
                                                 reads=[f"PT{pti}", mname], writes=[f"PT{pti}"])
                                    pendq.append((item, pti))
                                    if len(pendq) > 3:
                                        emit_pv(*pendq.pop(0))
                                while pendq:
                                    emit_pv(*pendq.pop(0))
                                flush_ev()

                            def selection_dve(c):
                                par = c % 2
                                o0 = ost[par][0]
                                r0 = rden[par]
                                osk = [gk('ost', par, 0, hh) for hh in range(4)]
                                P.op('dve', lambda: nc.vector.tensor_scalar(out=r0[:, 0, :, :], in0=o0[:, :, :, 64], scalar1=1e-30, scalar2=None, op0=ALU.max), reads=osk, writes=[gk('rden', par, 0)])
                                P.op('dve', lambda: nc.vector.reciprocal(out=r0[:, 0, :, :], in_=r0[:, 0, :, :]), reads=[gk('rden', par, 0)], writes=[gk('rden', par, 0)])
                                for hh in range(4):
                                    dst = imp if hh == 0 else itmp
                                    dk = gk('imp') if hh == 0 else gk('itmp')
                                    P.op('dve', lambda hh=hh, dst=dst: nc.vector.tensor_tensor(out=dst[:], in0=o0[:, hh, :, 65:97], in1=r0[:, 0, hh, :].unsqueeze(2).to_broadcast([128, 4, 32]), op=ALU.mult),
                                         reads=[gk('ost', par, 0, hh), gk('rden', par, 0)], writes=[dk])
                                    if hh > 0:
                                        P.op('dve', lambda: nc.vector.tensor_tensor(out=imp[:], in0=imp[:], in1=itmp[:], op=ALU.add), reads=[gk('imp'), gk('itmp')], writes=[gk('imp')])
                                P.op('dve', lambda: nc.vector.tensor_tensor(out=sc1[:], in0=imp[:], in1=selv[:, 4 * c:4 * c + 4, :], op=ALU.mult), reads=[gk('imp'), 'selv'], writes=[gk('sc1')])
                                P.op('dve', lambda: nc.vector.tensor_tensor(out=sc1[:], in0=sc1[:], in1=selb[:, 4 * c:4 * c + 4, :], op=ALU.add), reads=[gk('sc1'), 'selb'], writes=[gk('sc1')])
                                for j in range(4):
                                    P.op('dve', lambda j=j: nc.vector.max(out=m8[:, j, 0:8], in_=sc1[:, j, :]), reads=[gk('sc1')], writes=[gk('m8a', j)])
                                for j in range(4):
                                    P.op('dve', lambda j=j: nc.vector.match_replace(out=sc2[:, j, :], in_to_replace=m8[:, j, 0:8], in_values=sc1[:, j, :], imm_value=-3e38), reads=[gk('sc1'), gk('m8a', j)], writes=[gk('sc2', j)])
                                for j in range(4):
                                    P.op('dve', lambda j=j: nc.vector.max(out=m8[:, j, 8:16], in_=sc2[:, j, :]), reads=[gk('sc2', j)], writes=[gk('m8b', j)])
                                for j in range(4):
                                    P.op('dve', lambda j=j: nc.vector.tensor_scalar(out=negb[:, j, 64:96], in0=sc1[:, j, :], scalar1=m8[:, j, 15:16], scalar2=-1.0, op0=ALU.is_ge, op1=ALU.add),
                                         reads=[gk('sc1'), gk('m8b', j)], writes=[gk('negb', j)])

                            def negsel_T(c):
                                for j in range(4):
                                    P.op('pe', lambda j=j: nc.tensor.matmul(psX[0:96, j * 128:(j + 1) * 128], lhsT=negb[:, j, :], rhs=ident[:], start=True, stop=True),
                                         reads=[gk('negb', j), gk('negb0'), 'ident'], writes=[('psXn', j)])
                                for hh in range(4):
                                    if hh % 2 == 0:
                                        P.op('act', lambda hh=hh: nc.scalar.copy(out=q_aug[64:96, hh, c * 512:(c + 1) * 512], in_=psX[64:96, :]), reads=[('psXn', j) for j in range(4)], writes=[gk('qsel', hh, c)])
                                    else:
                                        P.op('dve', lambda hh=hh: nc.vector.tensor_copy(out=q_aug[64:96, hh, c * 512:(c + 1) * 512], in_=psX[64:96, :]), reads=[('psXn', j) for j in range(4)], writes=[gk('qsel', hh, c)])

                            def combine_dve(c):
                                par = c % 2
                                r = rden[par]
                                for br in (1, 2):
                                    P.op('dve', lambda br=br: nc.vector.reciprocal(out=r[:, br, :, :], in_=ost[par][br][:, :, :, 64]), reads=[gk('ost', par, br, hh) for hh in range(4)], writes=[gk('rden', par, br)])
                                for br in range(3):
                                    gv = gates[:, 4 * c:4 * c + 4, br * 8 + 4 * g:br * 8 + 4 * g + 4].rearrange("p j h -> p h j")
                                    P.op('dve', lambda br=br, gv=gv: nc.vector.tensor_tensor(out=r[:, br, :, :], in0=r[:, br, :, :], in1=gv, op=ALU.mult),
                                         reads=[gk('rden', par, br)] + [('gates', 4 * c + j) for j in range(4)], writes=[gk('rden', par, br)])
                                ov = oacc[:].rearrange("p j (h d) -> p h j d", h=4)
                                tv = otmp[:].rearrange("p j (h d) -> p h j d", h=4)
                                for br in range(3):
                                    dstv = ov if br == 0 else tv
                                    dk = gk('oacc') if br == 0 else gk('otmp')
                                    P.op('pool', lambda br=br, dstv=dstv: nc.gpsimd.tensor_tensor(out=dstv, in0=ost[par][br][:, :, :, 0:64], in1=r[:, br, :, :].unsqueeze(3).to_broadcast([128, 4, 4, 64]), op=ALU.mult),
                                         reads=[gk('ost', par, br, hh) for hh in range(4)] + [gk('rden', par, br)], writes=[dk])
                                    if br > 0:
                                        P.op('pool', lambda: nc.gpsimd.tensor_tensor(out=oacc[:], in0=oacc[:], in1=otmp[:], op=ALU.add), reads=[gk('oacc'), gk('otmp')], writes=[gk('oacc')])
                                P.op('act', lambda: nc.scalar.copy(out=ob[:], in_=oacc[:]), reads=[gk('oacc')], writes=[gk('ob')])

                            def otrans(c):
                                for j in range(4):
                                    for f in range(2):
                                        P.op('pe', lambda j=j, f=f: nc.tensor.transpose(out=psT[:, (j * 2 + f) * 128:(j * 2 + f + 1) * 128], in_=ob[:, j, f * 128:(f + 1) * 128], identity=ident[:]),
                                             reads=[gk('ob'), 'ident'], writes=['psT'])
                                P.op('dve', lambda: nc.vector.tensor_copy(
                                    out=mixT[:, 2 * g:2 * g + 2, c * 512:(c + 1) * 512].rearrange("p f (j t) -> p j f t", j=4),
                                    in_=psT[:].rearrange("p (j f t) -> p j f t", j=4, f=2)),
                                    reads=['psT'], writes=[('mixT', l, 2 * g, c), ('mixT', l, 2 * g + 1, c)])

                            run_branch(0, 0)
                            selection_dve(0)
                            for c in range(4):
                                if c == 0:
                                    run_branch(0, 2)
                                negsel_T(c)
                                run_branch(c, 1)
                                if c + 1 < 4:
                                    run_branch(c + 1, 0)
                                    selection_dve(c + 1)
                                combine_dve(c)
                                if c + 1 < 4:
                                    run_branch(c + 1, 2)
                                otrans(c)
                            rel = [gk('ksE'), gk('negb0'), gk('kwpad')] + [gk('q', hh, tc) for hh in range(4) for tc in range(4)] + [gk('qsel', hh, tc) for hh in range(4) for tc in range(4)]
                            rel += [gk('ks', tc) for tc in range(4)] + [gk('kw', tc) for tc in range(4)] + [f"PT{i}" for i in range(NPT)]
                            rel += [gk('ost', par, br, hh) for par in range(2) for br in range(3) for hh in range(4)] + [gk('rden', par, br) for par in range(2) for br in range(3)]
                            rel += [gk(n, j) for n in ('sc2', 'm8a', 'm8b', 'negb') for j in range(4)] + [gk(n) for n in ('imp', 'itmp', 'sc1', 'oacc', 'otmp', 'ob')]
                            P.release(rel, scratch[:, 3:4])
                    if stop == "att":
                        dbg_out['mixT'] = (mixT[:], MIXT, [128, KC, S], BF16)
                        finish()
                        return nc

                    with ExitStack() as so:
                        hx = [sbm(f"hx{i}", [128, D], F32, so) for i in range(8)]
                        load_ln(1 + 2 * l)
                        woctr = 0
                        groups = []
                        for gi in range(5):
                            if gi < 4:
                                tiles = []
                                for k in range(4):
                                    tt = gi * 4 + k
                                    i = (gi % 2) * 4 + k
                                    hk = f"hx{i}_{l}"
                                    P.dma('sp', lambda tt=tt, i=i: nc.sync.dma_start(out=hx[i][:], in_=hres_d[tt * 128:(tt + 1) * 128, :]), reads=[('hres', tt)], writes=[hk])
                                    for half in range(2):
                                        oi = woctr % 2
                                        woctr += 1
                                        pO = psO[oi]
                                        for kc in range(KC):
                                            P.op('pe', lambda kc=kc, tt=tt, half=half, pO=pO: nc.tensor.matmul(
                                                pO[:, :], lhsT=mixT[:, kc, tt * 128:(tt + 1) * 128], rhs=wo[:, kc, half * 512:(half + 1) * 512], start=(kc == 0), stop=(kc == KC - 1)),
                                                reads=[('mixT', l, kc, tt // 4), ('wo', half)], writes=[f"psO{oi}"])
                                        P.op('dve', lambda i=i, half=half, pO=pO: nc.vector.scalar_tensor_tensor(
                                            out=hx[i][:, half * 512:(half + 1) * 512], in0=hx[i][:, half * 512:(half + 1) * 512], scalar=ALPHA, in1=pO[:, :], op0=ALU.mult, op1=ALU.add),
                                            reads=[hk, f"psO{oi}"], writes=[hk])
                                    tiles.append((hx[i][:], hk, tt))
                                groups.append((tiles, ln_stats(tiles)))
                            if gi >= 1:
                                ln_apply(groups[gi - 1][0], groups[gi - 1][1], hres_d)
                        P.release([f"hx{i}_{l}" for i in range(8)], scratch[:, 4:5])
                    P.release(MIXT + [('gates', tt) for tt in range(NT)] + [('vs', tt) for tt in range(NT)] + [('vw', tt) for tt in range(NT)] +
                              ['vs_ones', 'vw_ones', 'kccT', 'vcc', 'convw', ('wo', 0), ('wo', 1)] + [(f"wst{i}", k) for i in range(2) for k in range(3)], scratch[:, 5:6])
                if stop == "mixer":
                    dbg_out['hT'] = (hT[:], HT_ALL, [128, KC, S], BF16)
                    finish()
                    return nc

                with ExitStack() as sf:
                    def sbf(name, shape, dt=F32):
                        return sf.enter_context(nc.sbuf_tensor(f"sb_{name}_{l}", list(shape), dt))
                    hacc = sbf("hacc", [128, NT, D])
                    wgs = [sbf(f"wgs{i}", [128, KC, 512], BF16) for i in range(2)]
                    wus = [sbf(f"wus{i}", [128, KC, 512], BF16) for i in range(2)]
                    wds = [sbf(f"wds{i}", [128, 4, D], BF16) for i in range(2)]
                    actb = [sbf(f"actb{i}", [128, 4, 512], BF16) for i in range(2)]
                    sil = [sbf(f"sil{i}", [128, 512], F32) for i in range(2)]
                    moe = (l % 2 == 1)
                    fk = lambda name, *a: (name, l) + a
                    for tt in range(NT):
                        P.dma('sp', lambda tt=tt: nc.sync.dma_start(out=hacc[:, tt, :], in_=hres_d[tt * 128:(tt + 1) * 128, :]), reads=[('hres', tt)], writes=[fk('hacc', tt)])
                        P.op('act', lambda tt=tt: nc.scalar.mul(out=hacc[:, tt, :], in_=hacc[:, tt, :], mul=ALPHA), reads=[fk('hacc', tt)], writes=[fk('hacc', tt)])
                    if moe:
                        wrs = sbf("wrs", [128, KC, NE], BF16)
                        gate = sbf("gate", [128, NT, NE])
                        lg = sbf("lg", [128, NT, NE])
                        ex = sbf("ex", [128, NT, NE])
                        mx8 = sbf("mx8", [128, NT, 8])
                        nm1 = sbf("nm1", [128, NT, 2])
                        P.dma('pool', lambda: nc.gpsimd.dma_start(out=wrs[:], in_=wr_d[0].rearrange("(kc p) n -> p kc n", p=128)), writes=['wrs'])
                        for tt in range(NT):
                            for kc in range(KC):
                                P.op('pe', lambda kc=kc, tt=tt: nc.tensor.matmul(psX[:, 384 + 0:384 + NE], lhsT=hT[:, kc, tt * 128:(tt + 1) * 128], rhs=wrs[:, kc, :], start=(kc == 0), stop=(kc == KC - 1)),
                                     reads=['wrs', ('hT', tt)], writes=['psXr'])
                            P.op('dve', lambda tt=tt: nc.vector.tensor_copy(out=lg[:, tt, :], in_=psX[:, 384:384 + NE]), reads=['psXr'], writes=[fk('lg', tt)])
                            P.op('dve', lambda tt=tt: nc.vector.max(out=mx8[:, tt, :], in_=lg[:, tt, :]), reads=[fk('lg', tt)], writes=[fk('mx8', tt)])
                            P.op('dve', lambda tt=tt: nc.vector.tensor_scalar(out=nm1[:, tt, 0:1], in0=mx8[:, tt, 0:1], scalar1=-1.0, scalar2=None, op0=ALU.mult), reads=[fk('mx8', tt)], writes=[fk('nm1', tt)])
                            P.op('act', lambda tt=tt: nc.scalar.activation(out=ex[:, tt, :], in_=lg[:, tt, :], func=AF.Exp, bias=nm1[:, tt, 0:1], scale=1.0), reads=[fk('lg', tt), fk('nm1', tt)], writes=[fk('ex', tt)])
                            P.op('dve', lambda tt=tt: nc.vector.scalar_tensor_tensor(out=ex[:, tt, :], in0=lg[:, tt, :], scalar=mx8[:, tt, 1:2], in1=ex[:, tt, :], op0=ALU.is_ge, op1=ALU.mult),
                                 reads=[fk('lg', tt), fk('mx8', tt), fk('ex', tt)], writes=[fk('ex', tt)])
                            P.op('dve', lambda tt=tt: nc.vector.tensor_reduce(out=nm1[:, tt, 1:2], in_=ex[:, tt, :], axis=mybir.AxisListType.X, op=ALU.add), reads=[fk('ex', tt)], writes=[fk('nm1b', tt)])
                            P.op('dve', lambda tt=tt: nc.vector.reciprocal(out=nm1[:, tt, 1:2], in_=nm1[:, tt, 1:2]), reads=[fk('nm1b', tt)], writes=[fk('nm1b', tt)])
                            P.op('dve', lambda tt=tt: nc.vector.tensor_scalar(out=gate[:, tt, :], in0=ex[:, tt, :], scalar1=nm1[:, tt, 1:2], scalar2=None, op0=ALU.mult), reads=[fk('ex', tt), fk('nm1b', tt)], writes=[fk('gate', tt)])
                        units = [(e, c0, n) for e in range(NE) for (c0, n) in _units(D_FFE // 128, 4)]
                    else:
                        units = [(None, c0, n) for (c0, n) in _units(D_FF // 128, 4)]
                    actr = 0
                    sctr = 0
                    octr2 = 0
                    load_ln(2 + 2 * l)
                    final = last_layer or stop == "ffn"
                    for ui, (e, c0, nch) in enumerate(units):
                        b = ui % 2
                        if moe:
                            g_src = mwg_d[0, e].rearrange("(kc p) n -> p kc n", p=128)
                            u_src = mwu_d[0, e].rearrange("(kc p) n -> p kc n", p=128)
                            d_src = mwd_d[0, e]
                        else:
                            g_src = wg_d[0].rearrange("(kc p) n -> p kc n", p=128)
                            u_src = wu_d[0].rearrange("(kc p) n -> p kc n", p=128)
                            d_src = wd_d[0]
                        ncol = nch * 128
                        f0 = c0 * 128
                        P.dma('pool', lambda b=b, g_src=g_src, f0=f0, ncol=ncol: nc.gpsimd.dma_start(out=wgs[b][:, :, 0:ncol], in_=g_src[:, :, f0:f0 + ncol]), writes=[fk('wgs', b)])
                        P.dma('pool', lambda b=b, u_src=u_src, f0=f0, ncol=ncol: nc.gpsimd.dma_start(out=wus[b][:, :, 0:ncol], in_=u_src[:, :, f0:f0 + ncol]), writes=[fk('wus', b)])
                        P.dma('pool', lambda b=b, d_src=d_src, f0=f0, ncol=ncol, nch=nch: nc.gpsimd.dma_start(out=wds[b][:, 0:nch, :], in_=d_src[f0:f0 + ncol, :].rearrange("(c p) n -> p c n", p=128)), writes=[fk('wds', b)])
                        for tc in range(4):
                            ab = actr % 2
                            actr += 1
                            for ch in range(nch):
                                si = sctr % 2
                                sctr += 1
                                for kc in range(KC):
                                    P.op('pe', lambda kc=kc, tc=tc, si=si, ch=ch, b=b: nc.tensor.matmul(psS[si][:, :], lhsT=wgs[b][:, kc, ch * 128:(ch + 1) * 128], rhs=hT[:, kc, tc * 512:(tc + 1) * 512], start=(kc == 0), stop=(kc == KC - 1)),
                                         reads=[fk('wgs', b)] + HT_ALL[tc * 4:tc * 4 + 4], writes=[f"psS{si}"])
                                for kc in range(KC):
                                    P.op('pe', lambda kc=kc, tc=tc, si=si, ch=ch, b=b: nc.tensor.matmul(psU[si][:, :], lhsT=wus[b][:, kc, ch * 128:(ch + 1) * 128], rhs=hT[:, kc, tc * 512:(tc + 1) * 512], start=(kc == 0), stop=(kc == KC - 1)),
                                         reads=[fk('wus', b)] + HT_ALL[tc * 4:tc * 4 + 4], writes=[f"psU{si}"])
                                P.op('act', lambda si=si: nc.scalar.activation(out=sil[si][:], in_=psS[si][:], func=AF.Silu), reads=[f"psS{si}"], writes=[fk('sil', si)])
                                P.op('dve', lambda si=si, ab=ab, ch=ch: nc.vector.tensor_tensor(out=actb[ab][:, ch, :], in0=sil[si][:], in1=psU[si][:], op=ALU.mult), reads=[fk('sil', si), f"psU{si}"], writes=[fk('actb', ab, ch)])
                            for j in range(4):
                                tt = 4 * tc + j
                                for half in range(2):
                                    oi = octr2 % 2
                                    octr2 += 1
                                    for ch in range(nch):
                                        P.op('pe', lambda ch=ch, j=j, half=half, oi=oi, ab=ab, b=b: nc.tensor.matmul(psO[oi][:, :], lhsT=actb[ab][:, ch, j * 128:(j + 1) * 128], rhs=wds[b][:, ch, half * 512:(half + 1) * 512], start=(ch == 0), stop=(ch == nch - 1)),
                                             reads=[fk('actb', ab, ch), fk('wds', b)], writes=[f"psO{oi}"])
                                    if moe:
                                        P.op('dve', lambda tt=tt, half=half, oi=oi, e=e: nc.vector.scalar_tensor_tensor(out=hacc[:, tt, half * 512:(half + 1) * 512], in0=psO[oi][:, :], scalar=gate[:, tt, e:e + 1], in1=hacc[:, tt, half * 512:(half + 1) * 512], op0=ALU.mult, op1=ALU.add),
                                             reads=[f"psO{oi}", fk('gate', tt), fk('hacc', tt)], writes=[fk('hacc', tt)])
                                    else:
                                        P.op('dve', lambda tt=tt, half=half, oi=oi: nc.vector.tensor_tensor(out=hacc[:, tt, half * 512:(half + 1) * 512], in0=hacc[:, tt, half * 512:(half + 1) * 512], in1=psO[oi][:, :], op=ALU.add),
                                             reads=[f"psO{oi}", fk('hacc', tt)], writes=[fk('hacc', tt)])
                            if ui == len(units) - 1:
                                ln_group([(hacc[:, tt, :], fk('hacc', tt), tt) for tt in range(tc * 4, tc * 4 + 4)], y_d if final else hres_d)
                    rel = [fk('hacc', tt) for tt in range(NT)] + [fk(n, b) for n in ('wgs', 'wus', 'wds', 'sil') for b in range(2)] + [fk('actb', ab, ch) for ab in range(2) for ch in range(4)]
                    if moe:
                        rel += ['wrs'] + [fk(n, tt) for n in ('lg', 'mx8', 'nm1', 'nm1b', 'ex', 'gate') for tt in range(NT)]
                    P.release(rel, scratch[:, 6:7])
                if stop == "ffn":
                    dbg_out['hT'] = (hT[:], HT_ALL, [128, KC, S], BF16)
                    finish()
                    return nc
        except _Stop:
            pass
        finish()
    return nc


def _consts():
    ident = np.eye(128, dtype=np.float32)
    n = np.arange(128)[:, None]
    t = np.arange(S)[None, :]
    cmpmask = ((16 * n + 31 <= t) & (n < 127)).astype(np.float32)
    k = np.arange(128)[:, None]
    q = np.arange(128)[None, :]
    tric = (k <= q).astype(np.float32)
    trib = (q < k).astype(np.float32)
    emat = (np.arange(S)[None, :] // 64 == np.arange(32)[:, None]).astype(np.float32) * BIG
    tt = np.arange(S)[:, None]
    j = np.arange(32)[None, :]
    cur = tt // 64
    valid = j * 64 <= tt
    forced = (j == 0) | (j == cur) | (j == cur - 1)
    selb = np.where(forced, FORCE, np.where(valid, 0.0, NEG)).astype(np.float32)
    selv = (valid & ~forced).astype(np.float32)
    selb = np.ascontiguousarray(selb.reshape(NT, 128, 32).transpose(1, 0, 2))
    selv = np.ascontiguousarray(selv.reshape(NT, 128, 32).transpose(1, 0, 2))
    cs = np.arange(128)[:, None] * 16
    ss = np.arange(32)[None, :] * 64
    ovl = ((cs < ss + 64) & (cs + 32 > ss) & (np.arange(128)[:, None] < 127)).astype(np.float32)
    return dict(ident=ident, cmpmask=cmpmask, tric=tric, trib=trib, emat=emat, selb=selb, selv=selv, ovl=ovl)


def make_in_maps(inputs, cores):
    f = lambda a: np.ascontiguousarray(np.asarray(a, dtype=np.float32))
    x = f(inputs['x'])
    lnp = np.stack([f(inputs['ln_in_g']), f(inputs['ln_in_b']),
                    f(inputs['ln1_g'])[0], f(inputs['ln1_b'])[0], f(inputs['ln2_g'])[0], f(inputs['ln2_b'])[0],
                    f(inputs['ln1_g'])[1], f(inputs['ln1_b'])[1], f(inputs['ln2_g'])[1], f(inputs['ln2_b'])[1]], axis=0)
    posT = np.ascontiguousarray(f(inputs['cmp_pos']).transpose(0, 1, 3, 2))
    b1T = np.ascontiguousarray(f(inputs['cmp_b1']).reshape(DEPTH, 2, 2, 128).transpose(0, 1, 3, 2))
    b2 = f(inputs['cmp_b2'])
    b2c = np.ascontiguousarray(b2[:, :, :, None])
    b2r = np.ascontiguousarray(b2[:, 1, :])
    convT = np.ascontiguousarray(f(inputs['conv_w']).reshape(DEPTH, 3, 4, 128).transpose(0, 3, 2, 1))
    shared = dict(
        w_in=f(inputs['w_in']), cmp_w1=f(inputs['cmp_w1']), cmp_w2=f(inputs['cmp_w2']), w_o=f(inputs['w_o']),
        ffn_wg=f(inputs['ffn_wg']), ffn_wu=f(inputs['ffn_wu']), ffn_wd=f(inputs['ffn_wd']),
        moe_router=f(inputs['moe_router']), moe_wg=f(inputs['moe_wg']), moe_wu=f(inputs['moe_wu']), moe_wd=f(inputs['moe_wd']),
        lnp=np.ascontiguousarray(lnp), posT=posT, b1T=b1T, b2c=b2c, b2r=b2r, convT=convT)
    shared.update(_consts())
    maps = []
    for c in cores:
        m = dict(shared)
        m['x'] = np.ascontiguousarray(x[c])
        maps.append(m)
    return maps


_NC_CACHE = {}


def kernel(**inputs):
    cores = list(range(8))
    if 'all' not in _NC_CACHE:
        _NC_CACHE['all'] = build_nc("all")
    nc = _NC_CACHE['all']
    in_maps = make_in_maps(inputs, cores)
    res = run_bass_kernel_spmd(nc, in_maps, core_ids=cores)
    out = np.stack([np.asarray(r["y"], dtype=np.float32) for r in res.results], axis=0)
    return out
```

```python
import numpy as np
from contextlib import ExitStack
import concourse.bass as bass
import concourse.mybir as mybir
from concourse.bass_utils import run_bass_kernel_spmd

F32 = mybir.dt.float32
BF16 = mybir.dt.bfloat16
AF = mybir.ActivationFunctionType
ALU = mybir.AluOpType

S = 2048
D = 1024
NT = 16
KC = 8
DEPTH = 2
D_IN = 2840
D_FF = 2816
NE = 8
D_FFE = 1408
ALPHA = float((2 * DEPTH) ** 0.25)
LN_EPS = 1e-5
BIG = 16384.0
FORCE = 1e9
NEG = -1e30

C_Q, C_KC, C_VC, C_KS, C_VS, C_KW, C_VW, C_G, C_U, C_B, C_C = 0, 512, 640, 768, 896, 1024, 1152, 1280, 1304, 1816, 2328

EPOCH = 20000
NDMASEM = 12


import types as _types


def _freeze(fn):
    if fn.__closure__ is None:
        return fn
    cells = []
    for c in fn.__closure__:
        try:
            cells.append(_types.CellType(c.cell_contents))
        except ValueError:
            cells.append(c)
    g = _types.FunctionType(fn.__code__, fn.__globals__, fn.__name__, fn.__defaults__, tuple(cells))
    g.__kwdefaults__ = fn.__kwdefaults__
    return g


class Prog:
    def __init__(self, nc):
        self.nc = nc
        self.ops = []
        self.engs = {'pe': nc.tensor, 'act': nc.scalar, 'dve': nc.vector, 'pool': nc.gpsimd, 'sp': nc.sync}
        self.known = set()
        self.last_release = None

    def op(self, eng, fn, reads=(), writes=(), dma=False):
        if _DIS[0]:
            return None
        banks = []
        for k in tuple(reads) + tuple(writes):
            b = k if isinstance(k, str) else k[0]
            if isinstance(b, str) and b.startswith('ps'):
                bk = ('BANK', 'psX' if b.startswith('psX') else b)
                if bk not in banks:
                    banks.append(bk)
        writes = tuple(writes) + tuple(banks)
        extra = []
        for k in tuple(reads) + tuple(writes):
            if k not in self.known:
                self.known.add(k)
                if self.last_release is not None:
                    extra.append(('init', k, self.last_release))
        self.ops.append([eng, _freeze(fn), tuple(reads), tuple(writes), dma, extra])
        return len(self.ops) - 1

    def dma(self, queue, fn, reads=(), writes=()):
        return self.op(queue, fn, reads, writes, dma=True)

    def release(self, keys, scratch):
        if _DIS[0]:
            return
        nc = self.nc
        keys = [k for k in keys if k in self.known]
        i = self.op('dve', lambda: nc.vector.memset(scratch, 0.0), reads=(), writes=tuple(keys) + ('__rel__',))
        for k in keys:
            self.known.discard(k)
        self.last_release = i

    def emit(self):
        nc = self.nc
        ops = self.ops
        n = len(ops)
        last_writer = {}
        readers = {}
        deps = [None] * n
        needed = [False] * n
        for i, (eng, fn, reads, writes, dma, extra) in enumerate(ops):
            for (_, k, rel) in extra:
                if last_writer.get(k, -1) < rel:
                    last_writer[k] = rel
                    readers[k] = []
            d = set()
            for r in reads:
                w = last_writer.get(r)
                if w is not None:
                    d.add(w)
            for w_ in writes:
                w = last_writer.get(w_)
                if w is not None:
                    d.add(w)
                for rd in readers.get(w_, ()):
                    d.add(rd)
            dd = []
            for j in d:
                if j == i:
                    continue
                ej, dj = ops[j][0], ops[j][4]
                if not dj and not dma and ej == eng:
                    if eng == 'pe':
                        continue
                dd.append(j)
            deps[i] = dd
            for j in dd:
                needed[j] = True
            for w_ in writes:
                last_writer[w_] = i
                readers[w_] = []
            for r in reads:
                readers.setdefault(r, []).append(i)
        eng_sems = {}
        counts = {e: 0 for e in self.engs}
        dma_sems = {}
        dma_counts = {}
        signal = [None] * n
        waited = {e: {} for e in self.engs}

        def get_eng_sem(e, epoch):
            key = (e, epoch)
            if key not in eng_sems:
                eng_sems[key] = nc.alloc_semaphore(name=f"s_{e}_{epoch}")
            return eng_sems[key]

        def get_dma_sem(q, k):
            key = (q, k)
            if key not in dma_sems:
                dma_sems[key] = nc.alloc_semaphore(name=f"d_{q}_{k}")
            return dma_sems[key]

        def do_wait(e, sem, val):
            sid = id(sem)
            if waited[e].get(sid, 0) >= val:
                return
            waited[e][sid] = val
            self.engs[e].wait_ge(sem, val)

        for i, (eng, fn, reads, writes, dma, extra) in enumerate(ops):
            for j in deps[i]:
                s = signal[j]
                assert s is not None, (i, j)
                do_wait(eng, s[0], s[1])
            if dma:
                k = dma_counts.get(eng, 0)
                dma_counts[eng] = k + 1
                sem = get_dma_sem(eng, k % NDMASEM)
                val = 16 * (k // NDMASEM + 1)
                if val > 16:
                    do_wait(eng, sem, val - 16)
                inst = fn()
                inst.then_inc(sem, 16)
                signal[i] = (sem, val)
            else:
                inst = fn()
                if needed[i]:
                    c = counts[eng]
                    counts[eng] = c + 1
                    sem = get_eng_sem(eng, c // EPOCH)
                    val = c % EPOCH + 1
                    inst.then_inc(sem, 1)
                    signal[i] = (sem, val)
        self.signal = signal
        last = {}
        for s in signal:
            if s is not None:
                sid = id(s[0])
                if sid not in last or last[sid][1] < s[1]:
                    last[sid] = s
        for sem, val in last.values():
            self.engs['sp'].wait_ge(sem, val)


class _Stop(Exception):
    pass


import os as _os


_DIS = [False]


def chk(tag):
    if _os.environ.get("KSTOP") == tag:
        _DIS[0] = True


def _units(total_chunks, per):
    out = []
    c = 0
    while c < total_chunks:
        n = min(per, total_chunks - c)
        out.append((c, n))
        c += n
    return out


def build_nc(upto="all", dbg=()):
    nc = bass.Bass("TRN2", target_bir_lowering=False)
    P = Prog(nc)

    def din(name, shape, dt=F32):
        return nc.dram_tensor(name, list(shape), dt, kind="ExternalInput").ap()

    x_d = din("x", [S, D])
    w_in_d = din("w_in", [DEPTH, D, D_IN])
    w1_d = din("cmp_w1", [DEPTH, 2, 2048, 256])
    w2_d = din("cmp_w2", [DEPTH, 2, 256, 64])
    w_o_d = din("w_o", [DEPTH, D, D])
    wg_d = din("ffn_wg", [1, D, D_FF])
    wu_d = din("ffn_wu", [1, D, D_FF])
    wd_d = din("ffn_wd", [1, D_FF, D])
    wr_d = din("moe_router", [1, D, NE])
    mwg_d = din("moe_wg", [1, NE, D, D_FFE])
    mwu_d = din("moe_wu", [1, NE, D, D_FFE])
    mwd_d = din("moe_wd", [1, NE, D_FFE, D])
    lnp_d = din("lnp", [10, D])
    posT_d = din("posT", [DEPTH, 2, 64, 32])
    b1T_d = din("b1T", [DEPTH, 2, 128, 2])
    b2c_d = din("b2c", [DEPTH, 2, 64, 1])
    b2r_d = din("b2r", [DEPTH, 64])
    convT_d = din("convT", [DEPTH, 128, 4, 3])
    ident_d = din("ident", [128, 128])
    cmpmask_d = din("cmpmask", [128, S])
    tric_d = din("tric", [128, 128])
    trib_d = din("trib", [128, 128])
    emat_d = din("emat", [32, S])
    selb_d = din("selb", [128, NT, 32])
    selv_d = din("selv", [128, NT, 32])
    ovl_d = din("ovl", [128, 32])
    y_d = nc.dram_tensor("y", [S, D], F32, kind="ExternalOutput").ap()
    hres_d = nc.dram_tensor("hres", [S, D], F32).ap()
    dbg_out = {}

    es = ExitStack()
    with es:
        def sb(name, shape, dt=F32, stack=es):
            return stack.enter_context(nc.sbuf_tensor("sb_" + name, list(shape), dt))

        def ps(name, shape, dt=F32):
            return es.enter_context(nc.psum_tensor(name, list(shape), dt))

        psS = [ps(f"psS{i}", [128, 512]) for i in range(2)]
        psU = [ps(f"psU{i}", [128, 512]) for i in range(2)]
        psO = [ps(f"psO{i}", [128, 512]) for i in range(2)]
        psT = ps("psT", [128, 1024], BF16)
        psX = ps("psX", [128, 512])

        ident = sb("identb", [128, 128], BF16)
        hT = sb("hT", [128, KC, S], BF16)
        lnG = sb("lnG", [128, D])
        lnB = sb("lnB", [128, D])
        cmpmask = sb("cmpmask", [128, S], BF16)
        tric = sb("tric", [128, 128], BF16)
        trib = sb("trib", [128, 128], BF16)
        selb = sb("selb", [128, NT, 32])
        selv = sb("selv", [128, NT, 32])
        scratch = sb("scratch", [128, 8])

        P.dma('pool', lambda: nc.gpsimd.dma_start(out=ident[:], in_=ident_d), writes=['ident'])
        P.dma('pool', lambda: nc.gpsimd.dma_start(out=cmpmask[:], in_=cmpmask_d), writes=['cmpmask'])
        P.dma('pool', lambda: nc.gpsimd.dma_start(out=tric[:], in_=tric_d), writes=['tric'])
        P.dma('pool', lambda: nc.gpsimd.dma_start(out=trib[:], in_=trib_d), writes=['trib'])
        P.dma('sp', lambda: nc.sync.dma_start(out=selb[:], in_=selb_d), writes=['selb'])
        P.dma('sp', lambda: nc.sync.dma_start(out=selv[:], in_=selv_d), writes=['selv'])

        lnctr = [0]

        def load_ln(idx):
            P.dma('sp', lambda: nc.sync.dma_start(out=lnG[:], in_=lnp_d[2 * idx, :].partition_broadcast(128)), writes=['lnG'])
            P.dma('sp', lambda: nc.sync.dma_start(out=lnB[:], in_=lnp_d[2 * idx + 1, :].partition_broadcast(128)), writes=['lnB'])

        mvall = [sb(f"mvall{i}", [128, 4, 8]) for i in range(2)]
        stall = [sb(f"stall{i}", [128, 4, 2, 6]) for i in range(2)]
        hb3 = [sb(f"hb3_{i}", [128, D], BF16) for i in range(3)]
        lngctr = [0]
        hbctr = [0]

        def ln_stats(tiles):
            b = lngctr[0] % 2
            lngctr[0] += 1
            n = len(tiles)
            m, st = mvall[b], stall[b]
            for k, (xt, xkey, tt) in enumerate(tiles):
                for c in range(2):
                    P.op('dve', lambda c=c, k=k, xt=xt: nc.vector.bn_stats(out=st[:, k, c, :], in_=xt[:, c * 512:(c + 1) * 512]), reads=[xkey], writes=[('lst', b, k, c)])
                P.op('dve', lambda k=k: nc.vector.bn_aggr(out=m[:, k, 0:2], in_=st[:, k, :, :]), reads=[('lst', b, k, 0), ('lst', b, k, 1)], writes=[('lmv', b, k)])
            allmv = [('lmv', b, k) for k in range(n)]
            P.op('dve', lambda: nc.vector.tensor_scalar(out=m[:, 0:n, 2], in0=m[:, 0:n, 1], scalar1=LN_EPS, scalar2=None, op0=ALU.add), reads=allmv, writes=[('lmvb', b)])
            P.op('act', lambda: nc.scalar.activation(out=m[:, 0:n, 3], in_=m[:, 0:n, 2], func=AF.Sqrt), reads=[('lmvb', b)], writes=[('lmvc', b)])
            P.op('dve', lambda: nc.vector.reciprocal(out=m[:, 0:n, 4], in_=m[:, 0:n, 3]), reads=[('lmvc', b)], writes=[('lmvd', b)])
            P.op('dve', lambda: nc.vector.scalar_tensor_tensor(out=m[:, 0:n, 5], in0=m[:, 0:n, 0], scalar=-1.0, in1=m[:, 0:n, 4], op0=ALU.mult, op1=ALU.mult), reads=allmv + [('lmvd', b)], writes=[('lmve', b)])
            return b

        def ln_apply(tiles, b, dst, need_hT=True):
            m = mvall[b]
            hbs = []

            def stage_a(k):
                xt, xkey, tt = tiles[k]
                P.op('act', lambda: nc.scalar.activation(out=xt, in_=xt, func=AF.Identity, bias=m[:, k, 5:6], scale=m[:, k, 4:5]), reads=[xkey, ('lmvd', b), ('lmve', b)], writes=[xkey])
                P.op('dve', lambda: nc.vector.tensor_tensor(out=xt, in0=xt, in1=lnG[:], op=ALU.mult), reads=[xkey, 'lnG'], writes=[xkey])
                P.op('pool', lambda: nc.gpsimd.tensor_tensor(out=xt, in0=xt, in1=lnB[:], op=ALU.add), reads=[xkey, 'lnB'], writes=[xkey])

            def stage_b(k):
                xt, xkey, tt = tiles[k]
                hi = hbctr[0] % 3
                hbctr[0] += 1
                hbt, hk = hb3[hi], f"hb3_{hi}"
                P.dma('sp', lambda: nc.sync.dma_start(out=dst[tt * 128:(tt + 1) * 128, :], in_=xt), reads=[xkey], writes=[('hres', tt)])
                if not need_hT:
                    return
                P.op('act', lambda: nc.scalar.copy(out=hbt[:], in_=xt), reads=[xkey], writes=[hk])
                for kk in range(KC):
                    P.op('pe', lambda kk=kk: nc.tensor.transpose(out=psT[:, kk * 128:(kk + 1) * 128], in_=hbt[:, kk * 128:(kk + 1) * 128], identity=ident[:]), reads=[hk, 'ident'], writes=['psT'])
                P.op('dve', lambda: nc.vector.tensor_copy(out=hT[:, :, tt * 128:(tt + 1) * 128], in_=psT[:].rearrange("p (k t) -> p k t", k=KC)), reads=['psT'], writes=[('hT', tt)])

            n = len(tiles)
            for k in range(n + 2):
                if k < n:
                    stage_a(k)
                if 0 <= k - 2 < n:
                    stage_b(k - 2)

        def ln_group(tiles, dst, need_hT=True):
            ln_apply(tiles, ln_stats(tiles), dst, need_hT)

        HT_ALL = [('hT', tt) for tt in range(NT)]

        with ExitStack() as s0:
            xt = [sb(f"xt{i}", [128, D], stack=s0) for i in range(8)]
            load_ln(0)
            grp = []
            for gi in range(5):
                if gi < 4:
                    tiles = []
                    for k in range(4):
                        tt = gi * 4 + k
                        i = (gi % 2) * 4 + k
                        P.dma('sp', lambda tt=tt, i=i: nc.sync.dma_start(out=xt[i][:], in_=x_d[tt * 128:(tt + 1) * 128, :]), writes=[f"xt{i}"])
                        tiles.append((xt[i][:], f"xt{i}", tt))
                    grp.append((tiles, ln_stats(tiles)))
                if gi >= 1:
                    ln_apply(grp[gi - 1][0], grp[gi - 1][1], hres_d if upto != "ln0" else y_d)
            P.release([f"xt{i}" for i in range(8)], scratch[:, 0:1])

        def finish():
            if _DIS[0]:
                dbg_out.clear()
                _DIS[0] = False
            for name, (tile_ap, keys, shape, dt) in dbg_out.items():
                d = nc.dram_tensor("dbg_" + name, list(shape), dt, kind="ExternalOutput").ap()
                P.dma('sp', lambda d=d, tile_ap=tile_ap: nc.sync.dma_start(out=d, in_=tile_ap), reads=keys)
            P.emit()

        if upto == "ln0":
            if 'hT' in dbg:
                dbg_out['hT'] = (hT[:], HT_ALL, [128, KC, S], BF16)
            finish()
            return nc

        n_layers = DEPTH if upto in ("all",) else (1 if not upto.startswith("L1") else 2)
        try:
            for l in range(n_layers):
                last_layer = (l == DEPTH - 1)
                stop = upto.split(":")[1] if (":" in upto and l == n_layers - 1) else None
                with ExitStack() as sm:
                    mkeys = []

                    def sbm(name, shape, dt=F32, stack=sm):
                        return stack.enter_context(nc.sbuf_tensor(f"sb_{name}_{l}", list(shape), dt))

                    mixT = sbm("mixT", [128, KC, S], BF16)
                    gates = sbm("gates", [128, NT, 24])
                    vs_aug = sbm("vs_aug", [128, NT, 2, 65], BF16)
                    vw_aug = sbm("vw_aug", [128, NT, 2, 65], BF16)
                    wst = [sbm(f"wst{i}", [128, KC, 384], BF16) for i in range(2)]
                    kccT = sbm("kccT", [128, 2, 128], BF16)
                    vcc = sbm("vcc", [128, 2, 97], BF16)
                    convw = sbm("convw", [128, 4, 3])
                    MIXT = [('mixT', l, c, tc) for c in range(KC) for tc in range(4)]
                    wctr = [0]

                    def load_wblock(segs):
                        i = wctr[0] % 2
                        wctr[0] += 1
                        keys = [(f"wst{i}", k) for k in range(3)]
                        off = 0
                        offs = []
                        for si_, (c0, ncol) in enumerate(segs):
                            wk = [keys[si_]] if si_ < len(segs) - 1 else keys[si_:]
                            P.dma('pool', lambda c0=c0, ncol=ncol, off=off, i=i: nc.gpsimd.dma_start(
                                out=wst[i][:, :, off:off + ncol],
                                in_=w_in_d[l].rearrange("(kc p) n -> p kc n", p=128)[:, :, c0:c0 + ncol]), writes=wk)
                            offs.append(off)
                            off += ncol
                        return wst[i], keys, offs

                    pctr = [0]

                    def proj_fm(wt, wkey, off, M, evac):
                        for tc in range(4):
                            i = pctr[0] % 2
                            pctr[0] += 1
                            for kc in range(KC):
                                P.op('pe', lambda kc=kc, tc=tc, i=i: nc.tensor.matmul(
                                    psS[i][0:M, :], lhsT=wt[:, kc, off:off + M], rhs=hT[:, kc, tc * 512:(tc + 1) * 512],
                                    start=(kc == 0), stop=(kc == KC - 1)),
                                    reads=list(wkey) + HT_ALL[tc * 4:tc * 4 + 4], writes=[f"psS{i}"])
                            evac(tc, psS[i], f"psS{i}")

                    P.dma('sp', lambda: nc.sync.dma_start(out=convw[:], in_=convT_d[l]), writes=['convw'])
                    P.op('pool', lambda: nc.gpsimd.memset(vs_aug[:, :, :, 64:65], 1.0), writes=['vs_ones'])
                    P.op('pool', lambda: nc.gpsimd.memset(vw_aug[:, :, :, 64:65], 1.0), writes=['vw_ones'])

                    with ExitStack() as sc_:
                        kvT = sbm("kvT", [64, 4, S], BF16, sc_)
                        kcp = sbm("kcp", [64, 32, 128], BF16, sc_)
                        w1s = sbm("w1s", [64, 32, 256], BF16, sc_)
                        w2s = sbm("w2s", [128, 2, 64], BF16, sc_)
                        posT = sbm("posT", [64, 32], F32, sc_)
                        b1T = sbm("b1T", [128, 2], F32, sc_)
                        b2c = sbm("b2c", [64, 1], F32, sc_)
                        b2r = sbm("b2r", [128, 64], F32, sc_)
                        ovl = sbm("ovl", [128, 32], F32, sc_)
                        gx = sbm("gx", [128, 128], F32, sc_)
                        gu = sbm("gu", [128, 128], F32, sc_)
                        gs_ = sbm("gs", [128, 128], F32, sc_)
                        hact = sbm("hact", [128, 2, 128], BF16, sc_)
                        wt, wkey, offs = load_wblock([(C_KC, 256)])
                        for idx in range(4):
                            def evac(tc, pt, pk, idx=idx):
                                P.op('act', lambda: nc.scalar.copy(out=kvT[:, idx, tc * 512:(tc + 1) * 512], in_=pt[0:64, :]), reads=[pk], writes=[('kvT', idx, tc)])
                            proj_fm(wt, wkey, offs[0] + idx * 64, 64, evac)
                        chk('A')
                        P.op('pool', lambda: nc.gpsimd.memset(vcc[:], 0.0), writes=['vcc'])
                        P.op('pool', lambda: nc.gpsimd.memset(kccT[:], 0.0), writes=['kccT'])
                        P.op('pool', lambda: nc.gpsimd.memset(vcc[:, :, 64:65], 1.0), reads=[], writes=['vcc'])
                        P.dma('sp', lambda: nc.sync.dma_start(out=ovl[:], in_=ovl_d), writes=['ovl'])
                        P.op('act', lambda: nc.scalar.copy(out=vcc[:, 0, 65:97], in_=ovl[:]), reads=['ovl'], writes=['vcc'])
                        P.op('act', lambda: nc.scalar.copy(out=vcc[:, 1, 65:97], in_=ovl[:]), reads=['ovl'], writes=['vcc'])
                        P.dma('sp', lambda: nc.sync.dma_start(out=b2r[:], in_=b2r_d[l, :].partition_broadcast(128)), writes=['b2r'])
                        chk('B')
                        for kv in range(2):
                            for r4 in range(4):
                                P.dma('pool', lambda kv=kv, r4=r4: nc.gpsimd.dma_start(out=w1s[:, r4 * 8:(r4 + 1) * 8, :], in_=w1_d[l, kv].rearrange("(r d) n -> d r n", d=64)[:, r4 * 8:(r4 + 1) * 8, :]), writes=[('w1s', r4)])
                            P.dma('pool', lambda kv=kv: nc.gpsimd.dma_start(out=w2s[:], in_=w2_d[l, kv].rearrange("(c p) n -> p c n", p=128)), writes=['w2s'])
                            P.dma('sp', lambda kv=kv: nc.sync.dma_start(out=posT[:], in_=posT_d[l, kv]), writes=['posT'])
                            P.dma('sp', lambda kv=kv: nc.sync.dma_start(out=b1T[:], in_=b1T_d[l, kv]), writes=['b1T'])
                            P.dma('sp', lambda kv=kv: nc.sync.dma_start(out=b2c[:], in_=b2c_d[l, kv]), writes=['b2c'])
                            chk('C')
                            for g in range(2):
                                idx = kv * 2 + g
                                for r in range(32):
                                    P.op('dve', lambda r=r, idx=idx: nc.vector.tensor_scalar(
                                        out=kcp[:, r, 0:127], in0=kvT[:, idx, r:r + 16 * 126 + 1:16], scalar1=posT[:, r:r + 1], scalar2=None, op0=ALU.add),
                                        reads=[('kvT', idx, tc) for tc in range(4)] + ['posT'], writes=[('kcp', r)])
                                chk('D')
                                for hc in range(2):
                                    for r in range(32):
                                        P.op('pe', lambda r=r, hc=hc: nc.tensor.matmul(
                                            psX[:, 0:127], lhsT=w1s[:, r, hc * 128:(hc + 1) * 128], rhs=kcp[:, r, 0:127], start=(r == 0), stop=(r == 31)),
                                            reads=[('w1s', r // 8), ('kcp', r)], writes=['psX'])
                                    P.op('act', lambda hc=hc: nc.scalar.activation(out=gx[:, 0:127], in_=psX[:, 0:127], func=AF.Identity, bias=b1T[:, hc:hc + 1], scale=1.0), reads=['psX', 'b1T'], writes=['gx'])
                                    P.op('dve', lambda: nc.vector.tensor_tensor(out=gu[:, 0:127], in0=gx[:, 0:127], in1=gx[:, 0:127], op=ALU.mult), reads=['gx'], writes=['gu'])
                                    P.op('dve', lambda: nc.vector.tensor_scalar(out=gu[:, 0:127], in0=gu[:, 0:127], scalar1=0.044715, scalar2=1.0, op0=ALU.mult, op1=ALU.add), reads=['gu'], writes=['gu'])
                                    P.op('dve', lambda: nc.vector.tensor_tensor(out=gu[:, 0:127], in0=gu[:, 0:127], in1=gx[:, 0:127], op=ALU.mult), reads=['gu', 'gx'], writes=['gu'])
                                    P.op('act', lambda: nc.scalar.activation(out=gs_[:, 0:127], in_=gu[:, 0:127], func=AF.Sigmoid, scale=1.5957691216057308), reads=['gu'], writes=['gs'])
                                    P.op('dve', lambda hc=hc: nc.vector.tensor_tensor(out=hact[:, hc, 0:127], in0=gx[:, 0:127], in1=gs_[:, 0:127], op=ALU.mult), reads=['gx', 'gs'], writes=[('hact', hc)])
                                    chk('E')
                                chk('E2')
                                if kv == 0:
                                    for hc in range(2):
                                        P.op('pe', lambda hc=hc: nc.tensor.matmul(psX[0:64, 128:255], lhsT=w2s[:, hc, :], rhs=hact[:, hc, 0:127], start=(hc == 0), stop=(hc == 1)),
                                             reads=['w2s', ('hact', hc)], writes=['psXb'])
                                    chk('F1')
                                    P.op('dve', lambda g=g: nc.vector.tensor_scalar(out=kccT[0:64, g, 0:127], in0=psX[0:64, 128:255], scalar1=b2c[:, 0:1], scalar2=None, op0=ALU.add), reads=['psXb', 'b2c'], writes=['kccT'])
                                    chk('F')
                                else:
                                    for hc in range(2):
                                        P.op('pe', lambda hc=hc: nc.tensor.matmul(psX[0:127, 256:320], lhsT=hact[:, hc, 0:127], rhs=w2s[:, hc, :], start=(hc == 0), stop=(hc == 1)),
                                             reads=['w2s', ('hact', hc)], writes=['psXc'])
                                    P.op('dve', lambda g=g: nc.vector.tensor_tensor(out=vcc[0:127, g, 0:64], in0=psX[0:127, 256:320], in1=b2r[0:127, :], op=ALU.add), reads=['psXc', 'b2r'], writes=['vcc'])
                        if 'kvT' in dbg and stop == "cmp":
                            dbg_out['kvT'] = (kvT[:], [('kvT', i, tc) for i in range(4) for tc in range(4)], [64, 4, S], BF16)
                        if stop == "cmp":
                            dbg_out['kccT'] = (kccT[0:64, :, :], ['kccT'], [64, 2, 128], BF16)
                            dbg_out['vcc'] = (vcc[:], ['vcc'], [128, 2, 97], BF16)
                            finish()
                            return nc
                        P.release([('kvT', i, tc) for i in range(4) for tc in range(4)] + [('kcp', r) for r in range(32)] +
                                  [('w1s', 0), ('w1s', 1), ('w1s', 2), ('w1s', 3), 'w2s', 'posT', 'b1T', 'b2c', 'b2r', 'ovl', 'gx', 'gu', 'gs', ('hact', 0), ('hact', 1), 'psX', 'psXb', 'psXc'], scratch[:, 1:2])

                    with ExitStack() as sv:
                        zt = sbm("zt", [128, S + 2], F32, sv)
                        usb = [sbm(f"usb{i}", [128, 512], F32, sv) for i in range(2)]
                        yt = [sbm(f"yt{i}", [128, 512], F32, sv) for i in range(2)]
                        P.op('pool', lambda: nc.gpsimd.memset(zt[:, 0:2], 0.0), writes=['zt_halo'])
                        cctr = 0
                        for c in range(4):
                            wt, wkey, offs = load_wblock([(C_U + c * 128, 128), (C_B + c * 128, 128), (C_C + c * 128, 128)])
                            for tc in range(4):
                                i = cctr % 2
                                cctr += 1
                                for (pt, pk, off) in ((psS[i], f"psS{i}", offs[0]), (psU[i], f"psU{i}", offs[1]), (psO[i], f"psO{i}", offs[2])):
                                    for kc in range(KC):
                                        P.op('pe', lambda kc=kc, tc=tc, pt=pt, off=off: nc.tensor.matmul(
                                            pt[:, :], lhsT=wt[:, kc, off:off + 128], rhs=hT[:, kc, tc * 512:(tc + 1) * 512],
                                            start=(kc == 0), stop=(kc == KC - 1)),
                                            reads=list(wkey) + HT_ALL[tc * 4:tc * 4 + 4], writes=[pk])
                                zk = ('zt', tc)
                                zprev = [('zt', tc - 1)] if tc > 0 else ['zt_halo']
                                P.op('act', lambda i=i: nc.scalar.copy(out=usb[i][:], in_=psS[i][:]), reads=[f"psS{i}"], writes=[f"usb{i}"])
                                P.op('dve', lambda i=i, tc=tc: nc.vector.tensor_tensor(out=zt[:, 2 + tc * 512:2 + (tc + 1) * 512], in0=usb[i][:], in1=psO[i][:], op=ALU.mult),
                                     reads=[f"usb{i}", f"psO{i}"], writes=[zk])
                                P.op('dve', lambda i=i, tc=tc, c=c: nc.vector.tensor_scalar(out=yt[i][:], in0=zt[:, 2 + tc * 512:2 + (tc + 1) * 512], scalar1=convw[:, c, 2:3], scalar2=None, op0=ALU.mult),
                                     reads=[zk, 'convw'], writes=[f"yt{i}"])
                                P.op('dve', lambda i=i, tc=tc, c=c: nc.vector.scalar_tensor_tensor(out=yt[i][:], in0=zt[:, 1 + tc * 512:1 + (tc + 1) * 512], scalar=convw[:, c, 1:2], in1=yt[i][:], op0=ALU.mult, op1=ALU.add),
                                     reads=[zk, 'convw', f"yt{i}"] + zprev, writes=[f"yt{i}"])
                                P.op('dve', lambda i=i, tc=tc, c=c: nc.vector.scalar_tensor_tensor(out=yt[i][:], in0=zt[:, tc * 512:(tc + 1) * 512], scalar=convw[:, c, 0:1], in1=yt[i][:], op0=ALU.mult, op1=ALU.add),
                                     reads=[zk, 'convw', f"yt{i}"] + zprev, writes=[f"yt{i}"])
                                P.op('dve', lambda i=i, tc=tc, c=c: nc.vector.tensor_tensor(out=mixT[:, 4 + c, tc * 512:(tc + 1) * 512], in0=yt[i][:], in1=psU[i][:], op=ALU.mult),
                                     reads=[f"yt{i}", f"psU{i}"], writes=[('mixT', l, 4 + c, tc)])
                        P.release([('zt', tc) for tc in range(4)] + ['zt_halo', 'usb0', 'usb1', 'yt0', 'yt1'], scratch[:, 2:3])
                    if stop == "conv":
                        dbg_out['mixT'] = (mixT[:], MIXT[16:], [128, KC, S], BF16)
                        finish()
                        return nc

                    wt, wkey, offs = load_wblock([(C_VS, 128), (C_VW, 128), (C_G, 24)])
                    for tt in range(NT):
                        i = tt % 2
                        for kc in range(KC):
                            P.op('pe', lambda kc=kc, tt=tt, i=i: nc.tensor.matmul(
                                psS[i][:, 0:280], lhsT=hT[:, kc, tt * 128:(tt + 1) * 128], rhs=wt[:, kc, 0:280], start=(kc == 0), stop=(kc == KC - 1)),
                                reads=list(wkey) + [('hT', tt)], writes=[f"psS{i}"])
                        P.op('act', lambda tt=tt, i=i: nc.scalar.copy(out=vs_aug[:, tt, :, 0:64], in_=psS[i][:, 0:128].rearrange("p (g d) -> p g d", g=2)), reads=[f"psS{i}"], writes=[('vs', tt)])
                        P.op('dve', lambda tt=tt, i=i: nc.vector.tensor_copy(out=vw_aug[:, tt, :, 0:64], in_=psS[i][:, 128:256].rearrange("p (g d) -> p g d", g=2)), reads=[f"psS{i}"], writes=[('vw', tt)])
                        P.op('act', lambda tt=tt, i=i: nc.scalar.activation(out=gates[:, tt, :], in_=psS[i][:, 256:280], func=AF.Sigmoid), reads=[f"psS{i}"], writes=[('gates', tt)])

                    wo = sbm("wo", [128, KC, D], BF16)
                    gblocks = [load_wblock([(C_Q + 256 * g_, 256), (C_KS + 64 * g_, 64), (C_KW + 64 * g_, 64)]) for g_ in range(2)]
                    P.dma('pool', lambda: nc.gpsimd.dma_start(out=wo[:, :, 0:512], in_=w_o_d[l].rearrange("(kc p) n -> p kc n", p=128)[:, :, 0:512]), writes=[('wo', 0)])
                    P.dma('pool', lambda: nc.gpsimd.dma_start(out=wo[:, :, 512:1024], in_=w_o_d[l].rearrange("(kc p) n -> p kc n", p=128)[:, :, 512:1024]), writes=[('wo', 1)])
                    for g in range(2):
                        with ExitStack() as sg:
                            q_aug = sbm(f"q_aug{g}", [128, 4, S], BF16, sg)
                            ks_aug = sbm(f"ks_aug{g}", [128, S], BF16, sg)
                            kwT = sbm(f"kwT{g}", [128, S], BF16, sg)
                            NPT = 6
                            PT = [sbm(f"PT{g}_{i}", [128, 512], BF16, sg) for i in range(NPT)]
                            ost = [[sbm(f"ost{g}_{par}_{br}", [128, 4, 4, 97], F32, sg) for br in range(3)] for par in range(2)]
                            imp = sbm(f"imp{g}", [128, 4, 32], F32, sg)
                            itmp = sbm(f"itmp{g}", [128, 4, 32], F32, sg)
                            rden = [sbm(f"rden{g}_{par}", [128, 3, 4, 4], F32, sg) for par in range(2)]
                            sc1 = sbm(f"sc1{g}", [128, 4, 32], F32, sg)
                            sc2 = sbm(f"sc2{g}", [128, 4, 32], F32, sg)
                            m8 = sbm(f"m8{g}", [128, 4, 16], F32, sg)
                            negb = sbm(f"negb{g}", [128, 4, 96], BF16, sg)
                            oacc = sbm(f"oacc{g}", [128, 4, 256], F32, sg)
                            otmp = sbm(f"otmp{g}", [128, 4, 256], F32, sg)
                            ob = sbm(f"ob{g}", [128, 4, 256], BF16, sg)
                            gk = lambda name, *a: (name, l, g) + a
                            P.op('pool', lambda: nc.gpsimd.memset(ks_aug[64:128, :], 0.0), writes=[gk('ksE')])
                            P.op('pool', lambda: nc.gpsimd.memset(kwT[64:128, :], 0.0), writes=[gk('kwpad')])
                            P.op('pool', lambda: nc.gpsimd.memset(q_aug[64:128, :, :], 0.0), writes=[gk('qsel', hh, tc) for hh in range(4) for tc in range(4)])
                            P.dma('pool', lambda: nc.gpsimd.dma_start(out=ks_aug[64:96, :], in_=emat_d), writes=[gk('ksE')])
                            P.op('pool', lambda: nc.gpsimd.memset(negb[:, :, 0:64], 0.0), writes=[gk('negb0')])
                            wt, wkey, offs = gblocks[g]
                            for hh in range(4):
                                def evac(tc, pt, pk, hh=hh):
                                    if tc % 2 == 0:
                                        P.op('act', lambda: nc.scalar.copy(out=q_aug[0:64, hh, tc * 512:(tc + 1) * 512], in_=pt[0:64, :]), reads=[pk], writes=[gk('q', hh, tc)])
                                    else:
                                        P.op('dve', lambda: nc.vector.tensor_copy(out=q_aug[0:64, hh, tc * 512:(tc + 1) * 512], in_=pt[0:64, :]), reads=[pk], writes=[gk('q', hh, tc)])
                                proj_fm(wt, wkey, offs[0] + hh * 64, 64, evac)

                            def evac_ks(tc, pt, pk):
                                P.op('act', lambda: nc.scalar.copy(out=ks_aug[0:64, tc * 512:(tc + 1) * 512], in_=pt[0:64, :]), reads=[pk], writes=[gk('ks', tc)])
                            proj_fm(wt, wkey, offs[1], 64, evac_ks)

                            def evac_kw(tc, pt, pk):
                                P.op('act', lambda: nc.scalar.copy(out=kwT[0:64, tc * 512:(tc + 1) * 512], in_=pt[0:64, :]), reads=[pk], writes=[gk('kw', tc)])
                            proj_fm(wt, wkey, offs[2], 64, evac_kw)

                            stctr = [0]
                            ptctr = [0]
                            octr = [0]

                            def attn_items(c, br):
                                items = []
                                for hh in range(4):
                                    if br == 0:
                                        items.append((hh, 0, 0, 3, [('cmp', None)]))
                                    elif br == 1:
                                        for kt in range(4 * c + 4):
                                            i_ = kt - 4 * c
                                            jlo = max(0, i_)
                                            items.append((hh, kt, jlo, 3, [('tric', i_)] if i_ >= 0 else []))
                                    else:
                                        for kt in range(max(0, 4 * c - 4), 4 * c + 4):
                                            i_ = kt - 4 * c
                                            jlo = max(0, i_)
                                            jhi = min(3, i_ + 4)
                                            mk = []
                                            if i_ >= 0:
                                                mk.append(('tric', i_))
                                            if i_ + 4 <= 3:
                                                mk.append(('trib', i_ + 4))
                                            items.append((hh, kt, jlo, jhi, mk))
                                return items

                            def run_branch(c, br):
                                par = c % 2
                                items = attn_items(c, br)
                                first, lastk = {}, {}
                                for (hh, kt, jlo, jhi, mk) in items:
                                    for j in range(jlo, jhi + 1):
                                        first.setdefault((hh, j), kt)
                                        lastk[(hh, j)] = kt
                                nv = 97 if br == 0 else 65
                                pendq = []
                                obank = {}
                                evq = []

                                def emit_pv(item, pti):
                                    hh, kt, jlo, jhi, mk = item
                                    newbank = hh not in obank
                                    if newbank:
                                        flush_ev()
                                        obank[hh] = octr[0] % 2
                                        octr[0] += 1
                                    ob_i = obank[hh]
                                    pO = psO[ob_i]
                                    for j in range(jlo, jhi + 1):
                                        if br == 0:
                                            rhs = vcc[:, g, :]
                                            rk = ['vcc']
                                        elif br == 1:
                                            rhs = vs_aug[:, kt, g, :]
                                            rk = [('vs', kt), 'vs_ones']
                                        else:
                                            rhs = vw_aug[:, kt, g, :]
                                            rk = [('vw', kt), 'vw_ones']
                                        st_flag = newbank and (j == jlo)
                                        P.op('pe', lambda j=j, rhs=rhs, pO=pO, kt=kt, hh=hh, st_flag=st_flag: nc.tensor.matmul(
                                            pO[:, j * 97:j * 97 + nv], lhsT=PT[pti][:, j * 128:(j + 1) * 128], rhs=rhs,
                                            start=st_flag, stop=(kt == lastk[(hh, j)]), skip_group_check=True),
                                            reads=[f"PT{pti}"] + rk, writes=[f"psO{ob_i}"])
                                    if kt == max(lastk[(hh, j)] for j in range(4)):
                                        evq.append((pO, hh, ob_i))

                                def flush_ev():
                                    while evq:
                                        pO, hh, ob_i = evq.pop(0)
                                        P.op('dve', lambda pO=pO, hh=hh: nc.vector.tensor_copy(
                                            out=ost[par][br][:, hh, :, 0:nv], in_=pO[:, 0:388].rearrange("p (j n) -> p j n", j=4)[:, :, 0:nv]),
                                            reads=[f"psO{ob_i}"], writes=[gk('ost', par, br, hh)])

                                for item in items:
                                    hh, kt, jlo, jhi, mk = item
                                    si = stctr[0] % 4
                                    stctr[0] += 1
                                    pti = ptctr[0] % NPT
                                    ptctr[0] += 1
                                    c0, c1 = jlo * 128, (jhi + 1) * 128
                                    qc0 = c * 512
                                    if br == 0:
                                        lhsT, lk = kccT[:, g, :], ['kccT']
                                        rhs, rk = q_aug[:, hh, qc0 + c0:qc0 + c1], [gk('q', hh, c), gk('qsel', hh, c)]
                                    elif br == 1:
                                        lhsT, lk = ks_aug[:, kt * 128:(kt + 1) * 128], [gk('ks', kt // 4), gk('ksE')]
                                        rhs, rk = q_aug[:, hh, qc0 + c0:qc0 + c1], [gk('q', hh, c), gk('qsel', hh, c)]
                                    else:
                                        lhsT, lk = kwT[:, kt * 128:(kt + 1) * 128], [gk('kw', kt // 4), gk('kwpad')]
                                        rhs, rk = q_aug[:, hh, qc0 + c0:qc0 + c1], [gk('q', hh, c), gk('qsel', hh, c)]
                                    sbank = (psS + psU)[si]
                                    sname = ("psS0", "psS1", "psU0", "psU1")[si]
                                    P.op('pe', lambda lhsT=lhsT, rhs=rhs, sbank=sbank, c0=c0, c1=c1: nc.tensor.matmul(sbank[:, c0:c1], lhsT=lhsT, rhs=rhs, start=True, stop=True),
                                         reads=lk + rk, writes=[sname])
                                    P.op('act', lambda sbank=sbank, pti=pti, c0=c0, c1=c1: nc.scalar.activation(out=PT[pti][:, c0:c1], in_=sbank[:, c0:c1], func=AF.Exp, scale=0.125),
                                         reads=[sname], writes=[f"PT{pti}"])
                                    flush_ev()
                                    for (mname, mj) in mk:
                                        if mname == 'cmp':
                                            P.op('dve', lambda pti=pti: nc.vector.tensor_tensor(out=PT[pti][:], in0=PT[pti][:], in1=cmpmask[:, qc0:qc0 + 512], op=ALU.mult),
                                                 reads=[f"PT{pti}", 'cmpmask'], writes=[f"PT{pti}"])
                                        else:
                                            mt = tric if mname == 'tric' else trib
                                            P.op('dve', lambda pti=pti, mj=mj, mt=mt: nc.vector.tensor_tensor(out=PT[pti][:, mj * 128:(mj + 1) * 128], in0=PT[pti][:, mj * 128:(mj + 1) * 128], in1=mt[:], op=ALU.mult),
                                                 reads=[f"PT{pti}", mname], writes=[f"PT{pti}"])
                                    pendq.append((item, pti))
                                    if len(pendq) > 3:
                                        emit_pv(*pendq.pop(0))
                                while pendq:
                                    emit_pv(*pendq.pop(0))
                                flush_ev()

                            def selection_dve(c):
                                par = c % 2
                                o0 = ost[par][0]
                                r0 = rden[par]
                                osk = [gk('ost', par, 0, hh) for hh in range(4)]
                                P.op('dve', lambda: nc.vector.tensor_scalar(out=r0[:, 0, :, :], in0=o0[:, :, :, 64], scalar1=1e-30, scalar2=None, op0=ALU.max), reads=osk, writes=[gk('rden', par, 0)])
                                P.op('dve', lambda: nc.vector.reciprocal(out=r0[:, 0, :, :], in_=r0[:, 0, :, :]), reads=[gk('rden', par, 0)], writes=[gk('rden', par, 0)])
                                for hh in range(4):
                                    dst = imp if hh == 0 else itmp
                                    dk = gk('imp') if hh == 0 else gk('itmp')
                                    P.op('dve', lambda hh=hh, dst=dst: nc.vector.tensor_tensor(out=dst[:], in0=o0[:, hh, :, 65:97], in1=r0[:, 0, hh, :].unsqueeze(2).to_broadcast([128, 4, 32]), op=ALU.mult),
                                         reads=[gk('ost', par, 0, hh), gk('rden', par, 0)], writes=[dk])
                                    if hh > 0:
                                        P.op('dve', lambda: nc.vector.tensor_tensor(out=imp[:], in0=imp[:], in1=itmp[:], op=ALU.add), reads=[gk('imp'), gk('itmp')], writes=[gk('imp')])
                                P.op('dve', lambda: nc.vector.tensor_tensor(out=sc1[:], in0=imp[:], in1=selv[:, 4 * c:4 * c + 4, :], op=ALU.mult), reads=[gk('imp'), 'selv'], writes=[gk('sc1')])
                                P.op('dve', lambda: nc.vector.tensor_tensor(out=sc1[:], in0=sc1[:], in1=selb[:, 4 * c:4 * c + 4, :], op=ALU.add), reads=[gk('sc1'), 'selb'], writes=[gk('sc1')])
                                for j in range(4):
                                    P.op('dve', lambda j=j: nc.vector.max(out=m8[:, j, 0:8], in_=sc1[:, j, :]), reads=[gk('sc1')], writes=[gk('m8a', j)])
                                for j in range(4):
                                    P.op('dve', lambda j=j: nc.vector.match_replace(out=sc2[:, j, :], in_to_replace=m8[:, j, 0:8], in_values=sc1[:, j, :], imm_value=-3e38), reads=[gk('sc1'), gk('m8a', j)], writes=[gk('sc2', j)])
                                for j in range(4):
                                    P.op('dve', lambda j=j: nc.vector.max(out=m8[:, j, 8:16], in_=sc2[:, j, :]), reads=[gk('sc2', j)], writes=[gk('m8b', j)])
                                for j in range(4):
                                    P.op('dve', lambda j=j: nc.vector.tensor_scalar(out=negb[:, j, 64:96], in0=sc1[:, j, :], scalar1=m8[:, j, 15:16], scalar2=-1.0, op0=ALU.is_ge, op1=ALU.add),
                                         reads=[gk('sc1'), gk('m8b', j)], writes=[gk('negb', j)])

                            def negsel_T(c):
                                for j in range(4):
                                    P.op('pe', lambda j=j: nc.tensor.matmul(psX[0:96, j * 128:(j + 1) * 128], lhsT=negb[:, j, :], rhs=ident[:], start=True, stop=True),
                                         reads=[gk('negb', j), gk('negb0'), 'ident'], writes=[('psXn', j)])
                                for hh in range(4):
                                    if hh % 2 == 0:
                                        P.op('act', lambda hh=hh: nc.scalar.copy(out=q_aug[64:96, hh, c * 512:(c + 1) * 512], in_=psX[64:96, :]), reads=[('psXn', j) for j in range(4)], writes=[gk('qsel', hh, c)])
                                    else:
                                        P.op('dve', lambda hh=hh: nc.vector.tensor_copy(out=q_aug[64:96, hh, c * 512:(c + 1) * 512], in_=psX[64:96, :]), reads=[('psXn', j) for j in range(4)], writes=[gk('qsel', hh, c)])

                            def combine_dve(c):
                                par = c % 2
                                r = rden[par]
                                for br in (1, 2):
                                    P.op('dve', lambda br=br: nc.vector.reciprocal(out=r[:, br, :, :], in_=ost[par][br][:, :, :, 64]), reads=[gk('ost', par, br, hh) for hh in range(4)], writes=[gk('rden', par, br)])
                                for br in range(3):
                                    gv = gates[:, 4 * c:4 * c + 4, br * 8 + 4 * g:br * 8 + 4 * g + 4].rearrange("p j h -> p h j")
                                    P.op('dve', lambda br=br, gv=gv: nc.vector.tensor_tensor(out=r[:, br, :, :], in0=r[:, br, :, :], in1=gv, op=ALU.mult),
                                         reads=[gk('rden', par, br)] + [('gates', 4 * c + j) for j in range(4)], writes=[gk('rden', par, br)])
                                ov = oacc[:].rearrange("p j (h d) -> p h j d", h=4)
                                tv = otmp[:].rearrange("p j (h d) -> p h j d", h=4)
                                for br in range(3):
                                    dstv = ov if br == 0 else tv
                                    dk = gk('oacc') if br == 0 else gk('otmp')
                                    P.op('pool', lambda br=br, dstv=dstv: nc.gpsimd.tensor_tensor(out=dstv, in0=ost[par][br][:, :, :, 0:64], in1=r[:, br, :, :].unsqueeze(3).to_broadcast([128, 4, 4, 64]), op=ALU.mult),
                                         reads=[gk('ost', par, br, hh) for hh in range(4)] + [gk('rden', par, br)], writes=[dk])
                                    if br > 0:
                                        P.op('pool', lambda: nc.gpsimd.tensor_tensor(out=oacc[:], in0=oacc[:], in1=otmp[:], op=ALU.add), reads=[gk('oacc'), gk('otmp')], writes=[gk('oacc')])
                                P.op('act', lambda: nc.scalar.copy(out=ob[:], in_=oacc[:]), reads=[gk('oacc')], writes=[gk('ob')])

                            def otrans(c):
                                for j in range(4):
                                    for f in range(2):
                                        P.op('pe', lambda j=j, f=f: nc.tensor.transpose(out=psT[:, (j * 2 + f) * 128:(j * 2 + f + 1) * 128], in_=ob[:, j, f * 128:(f + 1) * 128], identity=ident[:]),
                                             reads=[gk('ob'), 'ident'], writes=['psT'])
                                P.op('dve', lambda: nc.vector.tensor_copy(
                                    out=mixT[:, 2 * g:2 * g + 2, c * 512:(c + 1) * 512].rearrange("p f (j t) -> p j f t", j=4),
                                    in_=psT[:].rearrange("p (j f t) -> p j f t", j=4, f=2)),
                                    reads=['psT'], writes=[('mixT', l, 2 * g, c), ('mixT', l, 2 * g + 1, c)])

                            run_branch(0, 0)
                            selection_dve(0)
                            for c in range(4):
                                if c == 0:
                                    run_branch(0, 2)
                                negsel_T(c)
                                run_branch(c, 1)
                                if c + 1 < 4:
                                    run_branch(c + 1, 0)
                                    selection_dve(c + 1)
                                combine_dve(c)
                                if c + 1 < 4:
                                    run_branch(c + 1, 2)
                                otrans(c)
                            rel = [gk('ksE'), gk('negb0'), gk('kwpad')] + [gk('q', hh, tc) for hh in range(4) for tc in range(4)] + [gk('qsel', hh, tc) for hh in range(4) for tc in range(4)]
                            rel += [gk('ks', tc) for tc in range(4)] + [gk('kw', tc) for tc in range(4)] + [f"PT{i}" for i in range(NPT)]
                            rel += [gk('ost', par, br, hh) for par in range(2) for br in range(3) for hh in range(4)] + [gk('rden', par, br) for par in range(2) for br in range(3)]
                            rel += [gk(n, j) for n in ('sc2', 'm8a', 'm8b', 'negb') for j in range(4)] + [gk(n) for n in ('imp', 'itmp', 'sc1', 'oacc', 'otmp', 'ob')]
                            P.release(rel, scratch[:, 3:4])
                    if stop == "att":
                        dbg_out['mixT'] = (mixT[:], MIXT, [128, KC, S], BF16)
                        finish()
                        return nc

                    with ExitStack() as so:
                        hx = [sbm(f"hx{i}", [128, D], F32, so) for i in range(8)]
                        load_ln(1 + 2 * l)
                        woctr = 0
                        groups = []
                        for gi in range(5):
                            if gi < 4:
                                tiles = []
                                for k in range(4):
                                    tt = gi * 4 + k
                                    i = (gi % 2) * 4 + k
                                    hk = f"hx{i}_{l}"
                                    P.dma('sp', lambda tt=tt, i=i: nc.sync.dma_start(out=hx[i][:], in_=hres_d[tt * 128:(tt + 1) * 128, :]), reads=[('hres', tt)], writes=[hk])
                                    for half in range(2):
                                        oi = woctr % 2
                                        woctr += 1
                                        pO = psO[oi]
                                        for kc in range(KC):
                                            P.op('pe', lambda kc=kc, tt=tt, half=half, pO=pO: nc.tensor.matmul(
                                                pO[:, :], lhsT=mixT[:, kc, tt * 128:(tt + 1) * 128], rhs=wo[:, kc, half * 512:(half + 1) * 512], start=(kc == 0), stop=(kc == KC - 1)),
                                                reads=[('mixT', l, kc, tt // 4), ('wo', half)], writes=[f"psO{oi}"])
                                        P.op('dve', lambda i=i, half=half, pO=pO: nc.vector.scalar_tensor_tensor(
                                            out=hx[i][:, half * 512:(half + 1) * 512], in0=hx[i][:, half * 512:(half + 1) * 512], scalar=ALPHA, in1=pO[:, :], op0=ALU.mult, op1=ALU.add),
                                            reads=[hk, f"psO{oi}"], writes=[hk])
                                    tiles.append((hx[i][:], hk, tt))
                                groups.append((tiles, ln_stats(tiles)))
                            if gi >= 1:
                                ln_apply(groups[gi - 1][0], groups[gi - 1][1], hres_d)
                        P.release([f"hx{i}_{l}" for i in range(8)], scratch[:, 4:5])
                    P.release(MIXT + [('gates', tt) for tt in range(NT)] + [('vs', tt) for tt in range(NT)] + [('vw', tt) for tt in range(NT)] +
                              ['vs_ones', 'vw_ones', 'kccT', 'vcc', 'convw', ('wo', 0), ('wo', 1)] + [(f"wst{i}", k) for i in range(2) for k in range(3)], scratch[:, 5:6])
                if stop == "mixer":
                    dbg_out['hT'] = (hT[:], HT_ALL, [128, KC, S], BF16)
                    finish()
                    return nc

                with ExitStack() as sf:
                    def sbf(name, shape, dt=F32):
                        return sf.enter_context(nc.sbuf_tensor(f"sb_{name}_{l}", list(shape), dt))
                    hacc = sbf("hacc", [128, NT, D])
                    wgs = [sbf(f"wgs{i}", [128, KC, 512], BF16) for i in range(2)]
                    wus = [sbf(f"wus{i}", [128, KC, 512], BF16) for i in range(2)]
                    wds = [sbf(f"wds{i}", [128, 4, D], BF16) for i in range(2)]
                    actb = [sbf(f"actb{i}", [128, 4, 512], BF16) for i in range(2)]
                    sil = [sbf(f"sil{i}", [128, 512], F32) for i in range(2)]
                    moe = (l % 2 == 1)
                    fk = lambda name, *a: (name, l) + a
                    for tt in range(NT):
                        P.dma('sp', lambda tt=tt: nc.sync.dma_start(out=hacc[:, tt, :], in_=hres_d[tt * 128:(tt + 1) * 128, :]), reads=[('hres', tt)], writes=[fk('hacc', tt)])
                        P.op('act', lambda tt=tt: nc.scalar.mul(out=hacc[:, tt, :], in_=hacc[:, tt, :], mul=ALPHA), reads=[fk('hacc', tt)], writes=[fk('hacc', tt)])
                    if moe:
                        wrs = sbf("wrs", [128, KC, NE], BF16)
                        gate = sbf("gate", [128, NT, NE])
                        lg = sbf("lg", [128, NT, NE])
                        ex = sbf("ex", [128, NT, NE])
                        mx8 = sbf("mx8", [128, NT, 8])
                        nm1 = sbf("nm1", [128, NT, 2])
                        P.dma('pool', lambda: nc.gpsimd.dma_start(out=wrs[:], in_=wr_d[0].rearrange("(kc p) n -> p kc n", p=128)), writes=['wrs'])
                        for tt in range(NT):
                            for kc in range(KC):
                                P.op('pe', lambda kc=kc, tt=tt: nc.tensor.matmul(psX[:, 384 + 0:384 + NE], lhsT=hT[:, kc, tt * 128:(tt + 1) * 128], rhs=wrs[:, kc, :], start=(kc == 0), stop=(kc == KC - 1)),
                                     reads=['wrs', ('hT', tt)], writes=['psXr'])
                            P.op('dve', lambda tt=tt: nc.vector.tensor_copy(out=lg[:, tt, :], in_=psX[:, 384:384 + NE]), reads=['psXr'], writes=[fk('lg', tt)])
                            P.op('dve', lambda tt=tt: nc.vector.max(out=mx8[:, tt, :], in_=lg[:, tt, :]), reads=[fk('lg', tt)], writes=[fk('mx8', tt)])
                            P.op('dve', lambda tt=tt: nc.vector.tensor_scalar(out=nm1[:, tt, 0:1], in0=mx8[:, tt, 0:1], scalar1=-1.0, scalar2=None, op0=ALU.mult), reads=[fk('mx8', tt)], writes=[fk('nm1', tt)])
                            P.op('act', lambda tt=tt: nc.scalar.activation(out=ex[:, tt, :], in_=lg[:, tt, :], func=AF.Exp, bias=nm1[:, tt, 0:1], scale=1.0), reads=[fk('lg', tt), fk('nm1', tt)], writes=[fk('ex', tt)])
                            P.op('dve', lambda tt=tt: nc.vector.scalar_tensor_tensor(out=ex[:, tt, :], in0=lg[:, tt, :], scalar=mx8[:, tt, 1:2], in1=ex[:, tt, :], op0=ALU.is_ge, op1=ALU.mult),
                                 reads=[fk('lg', tt), fk('mx8', tt), fk('ex', tt)], writes=[fk('ex', tt)])
                            P.op('dve', lambda tt=tt: nc.vector.tensor_reduce(out=nm1[:, tt, 1:2], in_=ex[:, tt, :], axis=mybir.AxisListType.X, op=ALU.add), reads=[fk('ex', tt)], writes=[fk('nm1b', tt)])
                            P.op('dve', lambda tt=tt: nc.vector.reciprocal(out=nm1[:, tt, 1:2], in_=nm1[:, tt, 1:2]), reads=[fk('nm1b', tt)], writes=[fk('nm1b', tt)])
                            P.op('dve', lambda tt=tt: nc.vector.tensor_scalar(out=gate[:, tt, :], in0=ex[:, tt, :], scalar1=nm1[:, tt, 1:2], scalar2=None, op0=ALU.mult), reads=[fk('ex', tt), fk('nm1b', tt)], writes=[fk('gate', tt)])
                        units = [(e, c0, n) for e in range(NE) for (c0, n) in _units(D_FFE // 128, 4)]
                    else:
                        units = [(None, c0, n) for (c0, n) in _units(D_FF // 128, 4)]
                    actr = 0
                    sctr = 0
                    octr2 = 0
                    load_ln(2 + 2 * l)
                    final = last_layer or stop == "ffn"
                    for ui, (e, c0, nch) in enumerate(units):
                        b = ui % 2
                        if moe:
                            g_src = mwg_d[0, e].rearrange("(kc p) n -> p kc n", p=128)
                            u_src = mwu_d[0, e].rearrange("(kc p) n -> p kc n", p=128)
                            d_src = mwd_d[0, e]
                        else:
                            g_src = wg_d[0].rearrange("(kc p) n -> p kc n", p=128)
                            u_src = wu_d[0].rearrange("(kc p) n -> p kc n", p=128)
                            d_src = wd_d[0]
                        ncol = nch * 128
                        f0 = c0 * 128
                        P.dma('pool', lambda b=b, g_src=g_src, f0=f0, ncol=ncol: nc.gpsimd.dma_start(out=wgs[b][:, :, 0:ncol], in_=g_src[:, :, f0:f0 + ncol]), writes=[fk('wgs', b)])
                        P.dma('pool', lambda b=b, u_src=u_src, f0=f0, ncol=ncol: nc.gpsimd.dma_start(out=wus[b][:, :, 0:ncol], in_=u_src[:, :, f0:f0 + ncol]), writes=[fk('wus', b)])
                        P.dma('pool', lambda b=b, d_src=d_src, f0=f0, ncol=ncol, nch=nch: nc.gpsimd.dma_start(out=wds[b][:, 0:nch, :], in_=d_src[f0:f0 + ncol, :].rearrange("(c p) n -> p c n", p=128)), writes=[fk('wds', b)])
                        for tc in range(4):
                            ab = actr % 2
                            actr += 1
                            for ch in range(nch):
                                si = sctr % 2
                                sctr += 1
                                for kc in range(KC):
                                    P.op('pe', lambda kc=kc, tc=tc, si=si, ch=ch, b=b: nc.tensor.matmul(psS[si][:, :], lhsT=wgs[b][:, kc, ch * 128:(ch + 1) * 128], rhs=hT[:, kc, tc * 512:(tc + 1) * 512], start=(kc == 0), stop=(kc == KC - 1)),
                                         reads=[fk('wgs', b)] + HT_ALL[tc * 4:tc * 4 + 4], writes=[f"psS{si}"])
                                for kc in range(KC):
                                    P.op('pe', lambda kc=kc, tc=tc, si=si, ch=ch, b=b: nc.tensor.matmul(psU[si][:, :], lhsT=wus[b][:, kc, ch * 128:(ch + 1) * 128], rhs=hT[:, kc, tc * 512:(tc + 1) * 512], start=(kc == 0), stop=(kc == KC - 1)),
                                         reads=[fk('wus', b)] + HT_ALL[tc * 4:tc * 4 + 4], writes=[f"psU{si}"])
                                P.op('act', lambda si=si: nc.scalar.activation(out=sil[si][:], in_=psS[si][:], func=AF.Silu), reads=[f"psS{si}"], writes=[fk('sil', si)])
                                P.op('dve', lambda si=si, ab=ab, ch=ch: nc.vector.tensor_tensor(out=actb[ab][:, ch, :], in0=sil[si][:], in1=psU[si][:], op=ALU.mult), reads=[fk('sil', si), f"psU{si}"], writes=[fk('actb', ab, ch)])
                            for j in range(4):
                                tt = 4 * tc + j
                                for half in range(2):
                                    oi = octr2 % 2
                                    octr2 += 1
                                    for ch in range(nch):
                                        P.op('pe', lambda ch=ch, j=j, half=half, oi=oi, ab=ab, b=b: nc.tensor.matmul(psO[oi][:, :], lhsT=actb[ab][:, ch, j * 128:(j + 1) * 128], rhs=wds[b][:, ch, half * 512:(half + 1) * 512], start=(ch == 0), stop=(ch == nch - 1)),
                                             reads=[fk('actb', ab, ch), fk('wds', b)], writes=[f"psO{oi}"])
                                    if moe:
                                        P.op('dve', lambda tt=tt, half=half, oi=oi, e=e: nc.vector.scalar_tensor_tensor(out=hacc[:, tt, half * 512:(half + 1) * 512], in0=psO[oi][:, :], scalar=gate[:, tt, e:e + 1], in1=hacc[:, tt, half * 512:(half + 1) * 512], op0=ALU.mult, op1=ALU.add),
                                             reads=[f"psO{oi}", fk('gate', tt), fk('hacc', tt)], writes=[fk('hacc', tt)])
                                    else:
                                        P.op('dve', lambda tt=tt, half=half, oi=oi: nc.vector.tensor_tensor(out=hacc[:, tt, half * 512:(half + 1) * 512], in0=hacc[:, tt, half * 512:(half + 1) * 512], in1=psO[oi][:, :], op=ALU.add),
                                             reads=[f"psO{oi}", fk('hacc', tt)], writes=[fk('hacc', tt)])
                            if ui == len(units) - 1:
                                ln_group([(hacc[:, tt, :], fk('hacc', tt), tt) for tt in range(tc * 4, tc * 4 + 4)], y_d if final else hres_d, need_hT=not (last_layer and upto == 'all'))
                    rel = [fk('hacc', tt) for tt in range(NT)] + [fk(n, b) for n in ('wgs', 'wus', 'wds', 'sil') for b in range(2)] + [fk('actb', ab, ch) for ab in range(2) for ch in range(4)]
                    if moe:
                        rel += ['wrs'] + [fk(n, tt) for n in ('lg', 'mx8', 'nm1', 'nm1b', 'ex', 'gate') for tt in range(NT)]
                    P.release(rel, scratch[:, 6:7])
                if stop == "ffn":
                    dbg_out['hT'] = (hT[:], HT_ALL, [128, KC, S], BF16)
                    finish()
                    return nc
        except _Stop:
            pass
        finish()
    return nc


def _consts():
    ident = np.eye(128, dtype=np.float32)
    n = np.arange(128)[:, None]
    t = np.arange(S)[None, :]
    cmpmask = ((16 * n + 31 <= t) & (n < 127)).astype(np.float32)
    k = np.arange(128)[:, None]
    q = np.arange(128)[None, :]
    tric = (k <= q).astype(np.float32)
    trib = (q < k).astype(np.float32)
    emat = (np.arange(S)[None, :] // 64 == np.arange(32)[:, None]).astype(np.float32) * BIG
    tt = np.arange(S)[:, None]
    j = np.arange(32)[None, :]
    cur = tt // 64
    valid = j * 64 <= tt
    forced = (j == 0) | (j == cur) | (j == cur - 1)
    selb = np.where(forced, FORCE, np.where(valid, 0.0, NEG)).astype(np.float32)
    selv = (valid & ~forced).astype(np.float32)
    selb = np.ascontiguousarray(selb.reshape(NT, 128, 32).transpose(1, 0, 2))
    selv = np.ascontiguousarray(selv.reshape(NT, 128, 32).transpose(1, 0, 2))
    cs = np.arange(128)[:, None] * 16
    ss = np.arange(32)[None, :] * 64
    ovl = ((cs < ss + 64) & (cs + 32 > ss) & (np.arange(128)[:, None] < 127)).astype(np.float32)
    return dict(ident=ident, cmpmask=cmpmask, tric=tric, trib=trib, emat=emat, selb=selb, selv=selv, ovl=ovl)


def make_in_maps(inputs, cores):
    f = lambda a: np.ascontiguousarray(np.asarray(a, dtype=np.float32))
    x = f(inputs['x'])
    lnp = np.stack([f(inputs['ln_in_g']), f(inputs['ln_in_b']),
                    f(inputs['ln1_g'])[0], f(inputs['ln1_b'])[0], f(inputs['ln2_g'])[0], f(inputs['ln2_b'])[0],
                    f(inputs['ln1_g'])[1], f(inputs['ln1_b'])[1], f(inputs['ln2_g'])[1], f(inputs['ln2_b'])[1]], axis=0)
    posT = np.ascontiguousarray(f(inputs['cmp_pos']).transpose(0, 1, 3, 2))
    b1T = np.ascontiguousarray(f(inputs['cmp_b1']).reshape(DEPTH, 2, 2, 128).transpose(0, 1, 3, 2))
    b2 = f(inputs['cmp_b2'])
    b2c = np.ascontiguousarray(b2[:, :, :, None])
    b2r = np.ascontiguousarray(b2[:, 1, :])
    convT = np.ascontiguousarray(f(inputs['conv_w']).reshape(DEPTH, 3, 4, 128).transpose(0, 3, 2, 1))
    shared = dict(
        w_in=f(inputs['w_in']), cmp_w1=f(inputs['cmp_w1']), cmp_w2=f(inputs['cmp_w2']), w_o=f(inputs['w_o']),
        ffn_wg=f(inputs['ffn_wg']), ffn_wu=f(inputs['ffn_wu']), ffn_wd=f(inputs['ffn_wd']),
        moe_router=f(inputs['moe_router']), moe_wg=f(inputs['moe_wg']), moe_wu=f(inputs['moe_wu']), moe_wd=f(inputs['moe_wd']),
        lnp=np.ascontiguousarray(lnp), posT=posT, b1T=b1T, b2c=b2c, b2r=b2r, convT=convT)
    shared.update(_consts())
    maps = []
    for c in cores:
        m = dict(shared)
        m['x'] = np.ascontiguousarray(x[c])
        maps.append(m)
    return maps


_NC_CACHE = {}


def kernel(**inputs):
    cores = list(range(8))
    if 'all' not in _NC_CACHE:
        _NC_CACHE['all'] = build_nc("all")
    nc = _NC_CACHE['all']
    in_maps = make_in_maps(inputs, cores)
    res = run_bass_kernel_spmd(nc, in_maps, core_ids=cores)
    out = np.stack([np.asarray(r["y"], dtype=np.float32) for r in res.results], axis=0)
    return out
```
